# Optimizing a Trainium2 kernel written in Bass

```python
import math
import jax, jax.numpy as jnp
from jax import lax
import numpy as np

D_MODEL = 2048
BATCH = 4
SEQ = 2048
DEPTH = 1

ATTN_HEADS = 8
ATTN_HEAD_DIM = 128
MOBA_BLOCK = 256
MOBA_TOPK = 3
MOBA_Q_CHUNK = 64
REL_BUCKETS = 32
REL_MAX_DIST = 128
RET_HEADS = 8
RET_KEY_DIM = 128
RET_VAL_DIM = 256
RET_CHUNK = 128
ROPE_BASE = 10000.0
FFN_DIM = 5632
CONV_WIDTH = 3
EPS = 1e-6

ATTN_WIDTH = ATTN_HEADS * ATTN_HEAD_DIM
RET_QK_WIDTH = RET_HEADS * RET_KEY_DIM
RET_V_WIDTH = RET_HEADS * RET_VAL_DIM
IN_SPLITS = (ATTN_WIDTH, ATTN_WIDTH, ATTN_WIDTH, RET_QK_WIDTH, RET_QK_WIDTH,
             RET_V_WIDTH, RET_V_WIDTH, D_MODEL, D_MODEL)
IN_WIDTH = sum(IN_SPLITS)
N_MOD = 6

kernel_name = "hybrid_moba_retention_block"


def rms_norm(x, g):
    xf = x.astype(jnp.float32)
    y = xf * lax.rsqrt(jnp.mean(xf * xf, axis=-1, keepdims=True) + EPS)
    return (y * g.astype(jnp.float32)).astype(x.dtype)


def t5_bucket(dist):
    n = jnp.maximum(dist, 0)
    max_exact = REL_BUCKETS // 2
    nf = jnp.maximum(n, 1).astype(jnp.float32)
    large = max_exact + (jnp.log(nf / max_exact) / math.log(REL_MAX_DIST / max_exact)
                         * (REL_BUCKETS - max_exact)).astype(jnp.int32)
    large = jnp.minimum(large, REL_BUCKETS - 1)
    return jnp.where(n < max_exact, n, large)


def rotary(x, pos):
    half = x.shape[-1] // 2
    freqs = jnp.power(ROPE_BASE, -jnp.arange(half, dtype=jnp.float32) / half)
    ang = pos.astype(jnp.float32)[:, None] * freqs[None, :]
    cos, sin = jnp.cos(ang), jnp.sin(ang)
    xf = x.astype(jnp.float32)
    x1, x2 = xf[..., :half], xf[..., half:]
    return jnp.concatenate([x1 * cos - x2 * sin, x2 * cos + x1 * sin], axis=-1).astype(x.dtype)


def moba_attention(q, k, v, rel_bias):
    B, H, S, hd = q.shape
    nb = -(-S // MOBA_BLOCK)
    s_pad = nb * MOBA_BLOCK
    ksel = min(MOBA_TOPK, nb)
    pad = ((0, 0), (0, 0), (0, s_pad - S), (0, 0))
    k_pad = jnp.pad(k, pad)
    v_pad = jnp.pad(v, pad)
    k_blocks = k_pad.reshape(B, H, nb, MOBA_BLOCK, hd)
    v_blocks = v_pad.reshape(B, H, nb, MOBA_BLOCK, hd)
    k_mean = jnp.mean(k_blocks.astype(jnp.float32), axis=3)
    table = rel_bias.T.astype(jnp.float32)
    scale = hd ** -0.5
    b_idx = jnp.arange(B)[:, None, None, None]
    h_idx = jnp.arange(H)[None, :, None, None]
    h_idx5 = jnp.arange(H)[None, :, None, None, None]
    blk = jnp.arange(MOBA_BLOCK)

    def chunk(ci):
        start = ci * MOBA_Q_CHUNK
        qc = lax.dynamic_slice_in_dim(q, start, MOBA_Q_CHUNK, axis=2)
        q_pos = start + jnp.arange(MOBA_Q_CHUNK)
        cur = start // MOBA_BLOCK
        gate = jnp.einsum('bhqd,bhnd->bhqn', qc.astype(jnp.float32), k_mean)
        gate = jnp.where(jnp.arange(nb) < cur, gate, -jnp.inf)
        _, idx = lax.top_k(gate, ksel)
        sel_ok = jnp.arange(ksel) < cur
        kg = k_blocks[b_idx, h_idx, idx]
        vg = v_blocks[b_idx, h_idx, idx]
        k_pos = idx[..., None] * MOBA_BLOCK + blk
        bias_sel = table[h_idx5, t5_bucket(q_pos[None, None, :, None, None] - k_pos)]
        s_sel = jnp.einsum('bhqd,bhqnkd->bhqnk', qc, kg).astype(jnp.float32) * scale + bias_sel
        s_sel = jnp.where(sel_ok[:, None], s_sel, -jnp.inf)
        ko = lax.dynamic_slice_in_dim(k_pad, cur * MOBA_BLOCK, MOBA_BLOCK, axis=2)
        vo = lax.dynamic_slice_in_dim(v_pad, cur * MOBA_BLOCK, MOBA_BLOCK, axis=2)
        dist_own = q_pos[:, None] - (cur * MOBA_BLOCK + blk)[None, :]
        s_own = (jnp.einsum('bhqd,bhkd->bhqk', qc, ko).astype(jnp.float32) * scale
                 + table[:, t5_bucket(dist_own)])
        s_own = jnp.where(dist_own >= 0, s_own, -jnp.inf)
        logits = jnp.concatenate(
            [s_sel.reshape(B, H, MOBA_Q_CHUNK, ksel * MOBA_BLOCK), s_own], axis=-1)
        p = jax.nn.softmax(logits, axis=-1).astype(v.dtype)
        p_sel = p[..., :ksel * MOBA_BLOCK].reshape(B, H, MOBA_Q_CHUNK, ksel, MOBA_BLOCK)
        p_own = p[..., ksel * MOBA_BLOCK:]
        return (jnp.einsum('bhqnk,bhqnkd->bhqd', p_sel, vg)
                + jnp.einsum('bhqk,bhkd->bhqd', p_own, vo))

    out = lax.map(chunk, jnp.arange(S // MOBA_Q_CHUNK))
    return out.transpose(1, 2, 0, 3, 4).reshape(B, H, S, hd)


def retention(q, k, v):
    B, H, S, dk = q.shape
    dv = v.shape[-1]
    C = RET_CHUNK
    n = S // C
    dt = q.dtype
    log_decay = jnp.log(1.0 - jnp.power(2.0, -5.0 - jnp.arange(H, dtype=jnp.float32)))
    i = jnp.arange(C, dtype=jnp.float32)
    diff = i[:, None] - i[None, :]
    ld = log_decay[:, None, None]
    inner_decay = jnp.where(diff >= 0, jnp.exp(ld * jnp.maximum(diff, 0.0)), 0.0)
    q_decay = jnp.exp(log_decay[:, None] * (i + 1.0))
    k_decay = jnp.exp(log_decay[:, None] * (C - 1.0 - i))
    chunk_decay = jnp.exp(log_decay * C).astype(dt)[None, :, None, None]
    qc = q.reshape(B, H, n, C, dk)
    kc = k.reshape(B, H, n, C, dk)
    vc = v.reshape(B, H, n, C, dv)
    scores = jnp.einsum('bhnid,bhnjd->bhnij', qc, kc) * inner_decay[:, None].astype(dt)
    inner = jnp.einsum('bhnij,bhnje->bhnie', scores, vc)
    kv = jnp.einsum('bhnjd,bhnje->nbhde', kc * k_decay[:, None, :, None].astype(dt), vc)

    def step(state, kv_n):
        return chunk_decay * state + kv_n, state

    _, prev = lax.scan(step, jnp.zeros((B, H, dk, dv), kv.dtype), kv)
    cross = jnp.einsum('bhnid,nbhde->bhnie', qc * q_decay[:, None, :, None].astype(dt), prev)
    return (inner + cross).reshape(B, H, S, dv)


def head_group_norm(y, g):
    B, H, S, dv = y.shape
    yf = y.astype(jnp.float32)
    mu = jnp.mean(yf, axis=-1, keepdims=True)
    var = jnp.mean(jnp.square(yf - mu), axis=-1, keepdims=True)
    yn = ((yf - mu) * lax.rsqrt(var + EPS)).transpose(0, 2, 1, 3).reshape(B, S, H * dv)
    return (yn * g.astype(jnp.float32)).astype(y.dtype)


def causal_depthwise_conv(u, w, b):
    C = u.shape[-1]
    y = lax.conv_general_dilated(u, w[:, None, :].astype(u.dtype), window_strides=(1,),
                                 padding=[(CONV_WIDTH - 1, 0)],
                                 dimension_numbers=('NWC', 'WIO', 'NWC'),
                                 feature_group_count=C)
    return y + b.astype(u.dtype)


def setup_inputs(seed: int = 0) -> dict:
    key = jax.random.key(seed)
    ks = jax.random.split(key, 18)
    f32 = jnp.float32
    L = DEPTH

    def nrm(k, shape, scale):
        return jax.random.normal(k, shape, f32) * scale

    return {
        "x": nrm(ks[0], (BATCH, SEQ, D_MODEL), 1.0),
        "c": nrm(ks[1], (BATCH, D_MODEL), 1.0),
        "w_ada": nrm(ks[2], (L, D_MODEL, N_MOD * D_MODEL), D_MODEL ** -0.5),
        "b_ada": nrm(ks[3], (L, N_MOD * D_MODEL), 0.01),
        "norm1_g": 1.0 + nrm(ks[4], (L, D_MODEL), 0.02),
        "w_in": nrm(ks[5], (L, D_MODEL, IN_WIDTH), D_MODEL ** -0.5),
        "q_norm_g": 1.0 + nrm(ks[6], (L, ATTN_HEAD_DIM), 0.02),
        "k_norm_g": 1.0 + nrm(ks[7], (L, ATTN_HEAD_DIM), 0.02),
        "rel_bias": nrm(ks[8], (REL_BUCKETS, ATTN_HEADS), 0.3),
        "ret_norm_g": 1.0 + nrm(ks[9], (L, RET_V_WIDTH), 0.02),
        "w_attn_br": nrm(ks[10], (L, ATTN_WIDTH, D_MODEL), ATTN_WIDTH ** -0.5),
        "w_ret_br": nrm(ks[11], (L, RET_V_WIDTH, D_MODEL), RET_V_WIDTH ** -0.5),
        "w_o": nrm(ks[12], (L, D_MODEL, D_MODEL), D_MODEL ** -0.5),
        "norm2_g": 1.0 + nrm(ks[13], (L, D_MODEL), 0.02),
        "w_up": nrm(ks[14], (L, D_MODEL, 2 * FFN_DIM), D_MODEL ** -0.5),
        "conv_w": nrm(ks[15], (L, CONV_WIDTH, 2 * FFN_DIM), CONV_WIDTH ** -0.5),
        "conv_b": nrm(ks[16], (L, 2 * FFN_DIM), 0.01),
        "w_down": nrm(ks[17], (L, FFN_DIM, D_MODEL), FFN_DIM ** -0.5),
    }


def reference(x, c, w_ada, b_ada, norm1_g, w_in, q_norm_g, k_norm_g, rel_bias,
              ret_norm_g, w_attn_br, w_ret_br, w_o, norm2_g, w_up, conv_w, conv_b, w_down):
    B, S, D = x.shape
    pos = jnp.arange(S)
    split_at = [int(s) for s in np.cumsum(IN_SPLITS)[:-1]]

    def heads(t, n_heads, hd):
        return t.reshape(B, S, n_heads, hd).transpose(0, 2, 1, 3)

    for layer in range(DEPTH):
        mod = jax.nn.silu(c) @ w_ada[layer] + b_ada[layer]
        shift1, scale1, gate1, shift2, scale2, gate2 = jnp.split(mod, N_MOD, axis=-1)

        h = rms_norm(x, norm1_g[layer]) * (1.0 + scale1[:, None, :]) + shift1[:, None, :]
        proj = h @ w_in[layer]
        qa, ka, va, qr, kr, vr, gr, ga_logit, gb_logit = jnp.split(proj, split_at, axis=-1)

        qa = rms_norm(heads(qa, ATTN_HEADS, ATTN_HEAD_DIM), q_norm_g[layer])
        ka = rms_norm(heads(ka, ATTN_HEADS, ATTN_HEAD_DIM), k_norm_g[layer])
        va = heads(va, ATTN_HEADS, ATTN_HEAD_DIM)
        ya = moba_attention(qa, ka, va, rel_bias)
        ya = ya.transpose(0, 2, 1, 3).reshape(B, S, ATTN_WIDTH) @ w_attn_br[layer]

        qr = rotary(heads(qr, RET_HEADS, RET_KEY_DIM), pos)
        kr = rotary(heads(kr, RET_HEADS, RET_KEY_DIM), pos) * (RET_KEY_DIM ** -0.5)
        vr = heads(vr, RET_HEADS, RET_VAL_DIM)
        yr = retention(qr, kr, vr)
        yr = (head_group_norm(yr, ret_norm_g[layer]) * jax.nn.silu(gr)) @ w_ret_br[layer]

        merged = jax.nn.sigmoid(ga_logit) * ya + jax.nn.sigmoid(gb_logit) * yr
        x = x + gate1[:, None, :] * (merged @ w_o[layer])

        h2 = rms_norm(x, norm2_g[layer]) * (1.0 + scale2[:, None, :]) + shift2[:, None, :]
        u = causal_depthwise_conv(h2 @ w_up[layer], conv_w[layer], conv_b[layer])
        val, gt = jnp.split(u, 2, axis=-1)
        x = x + gate2[:, None, :] * ((jax.nn.silu(gt) * val) @ w_down[layer])
    return x
```

```python
import contextlib
import math

import numpy as np
import concourse.bass as bass
import concourse.mybir as mybir
from concourse.bass_utils import run_bass_kernel_spmd

F32 = mybir.dt.float32
BF16 = mybir.dt.bfloat16
ALU = mybir.AluOpType
AF = mybir.ActivationFunctionType
AX = mybir.AxisListType

D = 2048
OWN = 1024
WIN = 2048
NKC = 16
NEG = -30000.0
EPS = 1e-6
FFN = 5632
NFC = 44
ATT_SCALE = 128 ** -0.5
ENGS = ("pe", "act", "dve", "pool", "sp")
GRAN = 512


def _esz(dt):
    return 4 if dt == F32 else 2


class Sched:
    def __init__(self, nc):
        self.nc = nc
        self.ops = {e: [] for e in ENGS}
        self.last_w = {}
        self.readers = {}
        self.dma_cnt = {}

    @staticmethod
    def keys(x):
        if isinstance(x, (str, tuple)):
            return [x]
        ap = x.ap
        esz = _esz(x.dtype)
        pstride = ap[0][0]
        off = x.offset % pstride if pstride > 0 else x.offset
        ext = 1
        for (st, cnt) in ap[1:]:
            ext += abs(st) * (cnt - 1)
        lo = off * esz
        hi = (off + ext) * esz
        name = x.tensor.name
        if name.startswith("ps"):
            return [(name, 0)]
        return [(name, g) for g in range(lo // GRAN, (hi - 1) // GRAN + 1)]

    def op(self, eng, fn, reads=(), writes=(), dma=None):
        idx = len(self.ops[eng])
        me = (eng, idx)
        deps = set()
        rk = [k for r in reads for k in self.keys(r)]
        wk = [k for w in writes for k in self.keys(w)]
        for k in rk:
            w = self.last_w.get(k)
            if w is not None:
                deps.add(w)
        for k in wk:
            w = self.last_w.get(k)
            if w is not None:
                deps.add(w)
            for r in self.readers.get(k, {}).items():
                deps.add(r)
        deps.discard(me)
        rec = dict(fn=fn, deps=deps, dma=dma, signal=False, cnt=None)
        if dma is not None:
            self.dma_cnt[dma] = self.dma_cnt.get(dma, 0) + 1
            rec["cnt"] = 16 * self.dma_cnt[dma]
            rec["signal"] = True
        self.ops[eng].append(rec)
        for k in rk:
            self.readers.setdefault(k, {})[eng] = idx
        for k in wk:
            self.last_w[k] = me
            self.readers[k] = {}
        return me

    def emit(self, final_wait=()):
        nc = self.nc
        ops = self.ops
        for e in ENGS:
            for rec in ops[e]:
                nd = set()
                for (de, di) in rec["deps"]:
                    drec = ops[de][di]
                    if de == e and drec["dma"] is None and e in ("pe", "sp"):
                        continue
                    nd.add((de, di))
                rec["deps"] = nd
                for (de, di) in nd:
                    if ops[de][di]["dma"] is None:
                        ops[de][di]["signal"] = True
        for e in ENGS:
            c = 0
            for rec in ops[e]:
                if rec["dma"] is None and rec["signal"]:
                    c += 1
                    rec["cnt"] = c
        with contextlib.ExitStack() as st:
            esem = {e: st.enter_context(nc.semaphore("s_" + e)) for e in ENGS}
            dsem = {k: st.enter_context(nc.semaphore("d_%d" % i))
                    for i, k in enumerate(self.dma_cnt)}
            block = st.enter_context(nc.Block())

            def run(e):
                def body(engobj):
                    waited = {}
                    for rec in ops[e]:
                        need = {}
                        for (de, di) in rec["deps"]:
                            drec = ops[de][di]
                            s = ("d", drec["dma"]) if drec["dma"] is not None else ("e", de)
                            need[s] = max(need.get(s, 0), drec["cnt"])
                        for s, v in need.items():
                            if waited.get(s, 0) >= v:
                                continue
                            waited[s] = v
                            sem = dsem[s[1]] if s[0] == "d" else esem[s[1]]
                            engobj.wait_ge(sem, v)
                        ins = rec["fn"](engobj)
                        if rec["dma"] is not None:
                            ins.then_inc(dsem[rec["dma"]], 16)
                        elif rec["signal"]:
                            ins.then_inc(esem[e], 1)
                    if e == "sp":
                        for k in final_wait:
                            engobj.wait_ge(dsem[k], 16 * self.dma_cnt[k])
                return body

            block.tensor(run("pe"))
            block.scalar(run("act"))
            block.vector(run("dve"))
            block.gpsimd(run("pool"))
            block.sync(run("sp"))


def _t5_bucket(dist):
    n = np.maximum(dist, 0)
    max_exact = 16
    nf = np.maximum(n, 1).astype(np.float32)
    large = max_exact + (np.log(nf / max_exact) / math.log(128 / max_exact) * (32 - max_exact)).astype(np.int32)
    large = np.minimum(large, 31)
    return np.where(n < max_exact, n, large)


def _const_tables(half):
    out = {}
    k = np.arange(128)[:, None]
    q = np.arange(256)[None, :]
    dists = [q - k, q - k - 128, q - k + 128]
    out["bt_bucket"] = [_t5_bucket(d) for d in dists]
    out["bt_valid"] = [d >= 0 for d in dists]
    vm = np.full((8, 8), NEG, np.float32)
    for qt in range(8):
        j = qt // 2
        for n in range(8):
            ok = (n < 4 + j) and (n >= 4 or half == 1)
            if ok:
                vm[qt, n] = 0.0
    out["vmask"] = np.ascontiguousarray(np.broadcast_to(vm.reshape(1, 64), (128, 64))).astype(np.float32)
    e8 = np.zeros((8, 8, 128), np.float32)
    for n in range(8):
        e8[n, n, :] = 1.0
    out["e8"] = e8.reshape(8, 1024)
    pos = (np.arange(WIN) - 1024 + half * 1024).astype(np.float32)
    freqs = np.power(np.float32(10000.0), -np.arange(64, dtype=np.float32) / 64).astype(np.float32)
    ang = (pos[None, :] * freqs[:, None]).astype(np.float32)
    cos = np.cos(ang).astype(np.float32)
    sin = np.sin(ang).astype(np.float32)
    out["cosT"] = np.concatenate([cos, cos], 0)
    out["sinT"] = np.concatenate([sin, -sin], 0)
    hh = np.arange(8, dtype=np.float32)
    log_decay = np.log(1.0 - np.power(2.0, -5.0 - hh)).astype(np.float32)
    i = np.arange(128, dtype=np.float32)
    diff = i[:, None] - i[None, :]
    inner = np.where(diff >= 0, np.exp(log_decay[:, None, None] * np.maximum(diff, 0.0)), 0.0)
    sc = np.float32(128 ** -0.5)
    out["decT"] = np.ascontiguousarray(inner.transpose(2, 0, 1) * sc).astype(np.float32)
    qd = np.exp(log_decay[:, None] * (i + 1.0)).astype(np.float32)
    out["qdec"] = np.ascontiguousarray(np.broadcast_to(qd[None], (128, 8, 128))).astype(np.float32)
    kd = np.exp(log_decay[:, None] * (127.0 - i)).astype(np.float32) * sc
    out["kdec"] = np.ascontiguousarray(kd.T).astype(np.float32)
    out["chunk_decay"] = [float(np.exp(np.float32(log_decay[h] * 128.0))) for h in range(8)]
    out["flag"] = np.full((128, 1), float(half), np.float32)
    return out


def _col(v, n):
    return np.ascontiguousarray(np.asarray(v, np.float32).reshape(n, 128).T)


def _bc(v):
    v = np.asarray(v, np.float32).reshape(1, -1)
    return np.ascontiguousarray(np.broadcast_to(v, (128, v.shape[1])))


def make_core_inputs(inp, core):
    b, half = core // 2, core % 2
    ct = _const_tables(half)
    x = np.asarray(inp["x"], np.float32)
    m = {}
    if half == 1:
        m["xw"] = np.ascontiguousarray(x[b])
    else:
        m["xw"] = np.ascontiguousarray(np.concatenate([np.zeros((1024, D), np.float32), x[b, :1024]], 0))
    m["c_col"] = _col(inp["c"][b], 16)
    m["w_ada"] = np.ascontiguousarray(inp["w_ada"][0])
    bada = np.asarray(inp["b_ada"][0], np.float32)
    m["b_col"] = _col(bada, 96)
    m["b_g1"] = _bc(bada[4096:6144])
    m["b_g2"] = _bc(bada[10240:12288])
    m["g1_col"] = _col(inp["norm1_g"][0], 16)
    m["g2_col"] = _col(inp["norm2_g"][0], 16)
    m["w_in"] = np.ascontiguousarray(inp["w_in"][0])
    m["qg_col"] = _col(inp["q_norm_g"][0], 1)
    m["kg_col"] = _col(inp["k_norm_g"][0], 1)
    rb = np.asarray(inp["rel_bias"], np.float32)
    bt = np.empty((128, 8, 3, 256), np.float32)
    for j in range(3):
        g = rb[ct["bt_bucket"][j]]
        g = np.where(ct["bt_valid"][j][:, :, None], g, np.float32(NEG))
        bt[:, :, j, :] = g.transpose(0, 2, 1)
    m["bt"] = bt
    m["c31"] = _bc(rb[31])
    m["vmask"] = ct["vmask"]
    m["e8"] = ct["e8"]
    m["rg_bc"] = _bc(inp["ret_norm_g"][0])
    m["cosT"] = ct["cosT"]
    m["sinT"] = ct["sinT"]
    m["decT"] = ct["decT"]
    m["qdec"] = ct["qdec"]
    m["kdec"] = ct["kdec"]
    m["flag"] = ct["flag"]
    m["w_attn_br"] = np.ascontiguousarray(inp["w_attn_br"][0])
    m["w_ret_br"] = np.ascontiguousarray(inp["w_ret_br"][0])
    m["w_o"] = np.ascontiguousarray(inp["w_o"][0])
    m["w_up"] = np.ascontiguousarray(inp["w_up"][0])
    cw = np.asarray(inp["conv_w"][0], np.float32)
    m["cw"] = np.ascontiguousarray(cw.reshape(3, 88, 128).transpose(2, 1, 0))
    m["cb"] = _col(inp["conv_b"][0], 88)
    m["w_down"] = np.ascontiguousarray(inp["w_down"][0])
    m["ident"] = np.eye(128, dtype=np.float32)
    return m


INPUT_SHAPES = {
    "xw": [2048, 2048], "c_col": [128, 16], "w_ada": [2048, 12288], "b_col": [128, 96],
    "b_g1": [128, 2048], "b_g2": [128, 2048], "g1_col": [128, 16], "g2_col": [128, 16],
    "w_in": [2048, 13312], "qg_col": [128, 1], "kg_col": [128, 1], "bt": [128, 8, 3, 256],
    "c31": [128, 8], "vmask": [128, 64], "e8": [8, 1024], "rg_bc": [128, 2048],
    "cosT": [128, 2048], "sinT": [128, 2048], "decT": [128, 8, 128], "qdec": [128, 8, 128],
    "kdec": [128, 8], "flag": [128, 1], "w_attn_br": [1024, 2048], "w_ret_br": [2048, 2048],
    "w_o": [2048, 2048], "w_up": [2048, 11264], "cw": [128, 88, 3], "cb": [128, 88],
    "w_down": [5632, 2048], "ident": [128, 128],
}


def build_program(stop_after=None, debug=()):
    nc = bass.Bass("TRN2", target_bir_lowering=False)
    class _LazyIn(dict):
        def __missing__(self, k):
            self[k] = nc.dram_tensor(k, INPUT_SHAPES[k], F32, kind="ExternalInput").ap()
            return self[k]
    din = _LazyIn()
    y = nc.dram_tensor("y", [OWN, D], F32, kind="ExternalOutput").ap()
    x1s = nc.dram_tensor("x1s", [OWN, D], F32, kind="Internal").ap()
    zTs = nc.dram_tensor("zTs", [16, 128, OWN], BF16, kind="Internal").ap()
    dbg_out = {}
    cd = _const_tables(1)["chunk_decay"]
    X1KEYS = [("x1s", d) for d in range(8)]

    with contextlib.ExitStack() as st:
        slab_t = st.enter_context(nc.sbuf_tensor("slab", [128, 4, NKC, 256], BF16))
        hT_t = st.enter_context(nc.sbuf_tensor("hT", [128, 32768], BF16))
        A_t = st.enter_context(nc.sbuf_tensor("A", [128, 49152], BF16))
        C_t = st.enter_context(nc.sbuf_tensor("C", [128, 4096], BF16))
        psb = [st.enter_context(nc.psum_tensor("ps%d" % i, [128, 512], F32)) for i in range(8)]
        S = Sched(nc)

        def carve(t, boff, dt, shape):
            n = 1
            for s in shape[1:]:
                n *= s
            nb = n * _esz(dt)
            v = t[:, boff // 2:(boff + nb) // 2]
            if dt == F32:
                v = v.bitcast(F32)
            if len(shape) == 3:
                v = v.rearrange("p (a b) -> p a b", a=shape[1])
            elif len(shape) == 4:
                v = v.rearrange("p (a b c) -> p a b c", a=shape[1], b=shape[2])
            if shape[0] != 128:
                v = v[0:shape[0]]
            return v

        class Arena:
            def __init__(self, t, size):
                self.t, self.size, self.off = t, size, 0

            def alloc(self, dt, shape):
                self.off = (self.off + 63) // 64 * 64
                v = carve(self.t, self.off, dt, shape)
                n = _esz(dt)
                for s in shape[1:]:
                    n *= s
                self.off += n
                assert self.off <= self.size, (self.off, self.size)
                return v

        CA = Arena(C_t, 8192)
        AA = Arena(A_t, 98304)
        HA = Arena(hT_t, 65536)

        ident32 = CA.alloc(F32, [128, 128])
        ident_bf = CA.alloc(BF16, [128, 128])
        ones_bf = CA.alloc(BF16, [128, 128])
        ccol = CA.alloc(F32, [128, 16])
        s_bf = CA.alloc(BF16, [128, 16])
        s_bf2 = CA.alloc(BF16, [128, 16, 2])
        bcol = CA.alloc(F32, [128, 96])
        modc = CA.alloc(F32, [128, 4, 16])
        g1c = CA.alloc(F32, [128, 16])
        g2c = CA.alloc(F32, [128, 16])
        A1c = CA.alloc(F32, [128, 16])
        A2c = CA.alloc(F32, [128, 16])
        qgc = CA.alloc(F32, [128, 1])
        kgc = CA.alloc(F32, [128, 1])
        c31 = CA.alloc(F32, [128, 8])
        vmask = CA.alloc(F32, [128, 64])
        kdec = CA.alloc(F32, [128, 8])
        flag = CA.alloc(F32, [128, 1])
        epsc = CA.alloc(F32, [128, 1])
        ssq = CA.alloc(F32, [128, 17])
        rstd = CA.alloc(F32, [128, 17])
        smallf = CA.alloc(F32, [128, 8])
        km = CA.alloc(F32, [128, 8])
        km_bf = CA.alloc(BF16, [128, 8])
        gm = CA.alloc(F32, [128, 64])
        mx8 = CA.alloc(F32, [128, 64])
        nm = CA.alloc(F32, [128, 64])
        nm_bf = CA.alloc(BF16, [128, 64])
        bnst = CA.alloc(F32, [128, 3, 6])
        bnmv = CA.alloc(F32, [128, 3, 2])
        qnh = CA.alloc(BF16, [128, 4, 2])
        yaTh = CA.alloc(BF16, [128, 8, 2])
        qrTh = CA.alloc(BF16, [128, 2, 2])
        sTh = CA.alloc(BF16, [128, 2])
        qdTh = CA.alloc(BF16, [128, 2])
        zTh = CA.alloc(BF16, [128, 16, 2])
        mTh = CA.alloc(BF16, [128, 16, 2])
        h2Th = CA.alloc(BF16, [128, 16, 2])
        Phs = CA.alloc(F32, [128, 2, 2])

        def dma_sp(out, in_, reads, writes, key):
            S.op("sp", lambda e: e.dma_start(out=out, in_=in_), reads=reads, writes=writes, dma=key)

        def dma_pool(out, in_, reads, writes, key):
            S.op("pool", lambda e: e.dma_start(out=out, in_=in_), reads=reads, writes=writes, dma=key)

        def load_const(dst, name, cast=False):
            if cast:
                dma_pool(dst, din[name], [], [dst], "c_" + name + "_c")
            else:
                dma_sp(dst, din[name], [], [dst], "c_" + name)

        def mm(out, lhsT, rhs, start, stop):
            S.op("pe", lambda e: e.matmul(out, lhsT, rhs, start=start, stop=stop),
                 reads=[lhsT, rhs], writes=[out])

        def tp(out, in_, ident):
            S.op("pe", lambda e: e.matmul(out, in_, ident, start=True, stop=True), reads=[in_, ident], writes=[out])

        def act(out, in_, func, bias=None, scale=None, accum=None):
            kw = {}
            r = [in_]
            w = [out]
            if bias is not None:
                kw["bias"] = bias
                if not isinstance(bias, float):
                    r.append(bias)
            if scale is not None:
                kw["scale"] = scale
                if not isinstance(scale, float):
                    r.append(scale)
            if accum is not None:
                kw["accum_out"] = accum
                w.append(accum)
            S.op("act", lambda e: e.activation(out, in_, func, **kw), reads=r, writes=w)

        def ts(out, in0, s1, s2, op0, op1=None):
            r = [in0]
            if not isinstance(s1, float):
                r.append(s1)
            if s2 is not None and not isinstance(s2, float):
                r.append(s2)
            if op1 is None:
                S.op("dve", lambda e: e.tensor_scalar(out, in0, s1, None, op0), reads=r, writes=[out])
            else:
                S.op("dve", lambda e: e.tensor_scalar(out, in0, s1, s2, op0, op1), reads=r, writes=[out])

        def tt(out, in0, in1, op):
            S.op("dve", lambda e: e.tensor_tensor(out, in0, in1, op), reads=[in0, in1], writes=[out])

        def stt(out, in0, sc, in1, op0, op1):
            r = [in0, in1]
            if not isinstance(sc, float):
                r.append(sc)
            S.op("dve", lambda e: e.scalar_tensor_tensor(out, in0, sc, in1, op0, op1), reads=r, writes=[out])

        def recip(out, in_):
            S.op("dve", lambda e: e.reciprocal(out, in_), reads=[in_], writes=[out])

        def memset(ap, v):
            S.op("dve", lambda e: e.memset(ap, v), reads=[], writes=[ap])

        def acopy(out, in_):
            S.op("act", lambda e: e.copy(out, in_), reads=[in_], writes=[out])

        def vcopy(out, in_):
            S.op("dve", lambda e: e.tensor_copy(out, in_), reads=[in_], writes=[out])

        ev_tog = [0]

        def evac_affine(out, in_, sc, bi):
            ts(out, in_, sc, bi, ALU.mult, ALU.add)

        def evac_copy(out, in_):
            vcopy(out, in_)

        bank_ctr = [0]

        def bank():
            b = bank_ctr[0] % 8
            bank_ctr[0] += 1
            return psb[b][:]

        slab_ctr = [0]

        def slab_load(w, r0, nk, c0, ncols=256):
            i = slab_ctr[0] % 4
            slab_ctr[0] += 1
            dst = slab_t[:, i, 0:nk, 0:ncols]
            src = w[r0:r0 + nk * 128, c0:c0 + ncols].rearrange("(kc p) n -> p kc n", p=128)
            dma_pool(dst, src, [], [slab_t[:, i, :, :]], ("slab", i))
            return slab_t[:, i]

        def dump(name, ap, shape, dt):
            if name not in debug:
                return
            o = nc.dram_tensor("dbg_" + name, list(shape), dt, kind="ExternalOutput").ap()
            dbg_out[name] = o
            dma_sp(o, ap, [ap], ["dbg_" + name], "dbg_" + name)

        def finish(extra=()):
            fw = [k for k in extra] + ["dbg_" + k for k in dbg_out]
            S.emit(final_wait=fw)
            return nc, dbg_out

        load_const(ident32, "ident")
        load_const(ident_bf, "ident", cast=True)
        load_const(ccol, "c_col")
        load_const(bcol, "b_col")
        load_const(g1c, "g1_col")
        load_const(g2c, "g2_col")
        load_const(qgc, "qg_col")
        load_const(kgc, "kg_col")
        load_const(c31, "c31")
        load_const(vmask, "vmask")
        load_const(kdec, "kdec")
        load_const(flag, "flag")
        memset(ones_bf, 1.0)
        memset(epsc, EPS)
        act(s_bf, ccol, AF.Silu)
        vcopy(s_bf2[:, :, 0], s_bf)
        vcopy(s_bf2[:, :, 1], s_bf)

        def build_s_rep(s_rep):
            for kc in range(NKC):
                ts(s_rep[:, kc, :], ones_bf, s_bf[:, kc:kc + 1], None, ALU.mult)

        if stop_after == "0":
            dump("s_bf", s_bf, [128, 16], BF16)
            return finish()
        def mod_col_section(sec_in_w, sec_out):
            ps = bank()
            for j in range(8):
                sl = slab_load(din["w_ada"], 0, NKC, sec_in_w * 2048 + j * 256)
                for m in range(2):
                    col = j * 2 + m
                    for kc in range(NKC):
                        mm(ps[:, 2 * col:2 * col + 2], sl[:, kc, m * 128:(m + 1) * 128], s_bf2[:, kc, :],
                           kc == 0, kc == NKC - 1)
            tt(modc[:, sec_out, :], ps[:, 0:32].rearrange("p (a b) -> p a b", b=2)[:, :, 0],
               bcol[:, sec_in_w * 16:(sec_in_w + 1) * 16], ALU.add)

        def mod_row_section(sec_in_w, dst, s_rep):
            for j in range(8):
                sl = slab_load(din["w_ada"], 0, NKC, sec_in_w * 2048 + j * 256)
                ps = bank()
                for kc in range(NKC):
                    mm(ps[:, 0:256], s_rep[:, kc, :], sl[:, kc, :], kc == 0, kc == NKC - 1)
                evac_copy(dst[:, j * 256:(j + 1) * 256], ps[:, 0:256])

        import os as _os
        if _os.environ.get("SKIPA"):
            memset(modc[:, 0:2, :], 0.5)
        else:
            mod_col_section(1, 1)
            mod_col_section(0, 0)
        ts(A1c, modc[:, 1, :], 1.0, None, ALU.add)
        tt(A1c, A1c, g1c, ALU.mult)
        B1c = modc[:, 0, :]
        dump("modc", modc[:, 0:2, :], [128, 2, 16], F32)
        if stop_after == "A":
            return finish()

        hT = carve(hT_t, 0, BF16, [128, 2, NKC, 1024])

        def norm_tile(xt, junk, np_, si, Ac, Bc, dst_fn):
            act(junk[0:np_, :], xt, AF.Square, accum=ssq[0:np_, si:si + 1])
            act(rstd[0:np_, si:si + 1], ssq[0:np_, si:si + 1], AF.Sqrt, bias=epsc[0:np_, :], scale=1.0 / D)
            recip(rstd[0:np_, si:si + 1], rstd[0:np_, si:si + 1])
            ts(junk[0:np_, :], xt, rstd[0:np_, si:si + 1], None, ALU.mult)
            import os as _os
            if _os.environ.get("NB") == "1":
                dump("junk", junk, [128, 2048], BF16)
                return
            for q4 in range(int(_os.environ.get("NQ", "4"))):
                ps = bank()
                for j in range(4):
                    dc = q4 * 4 + j
                    tp(ps[:, j * 128:j * 128 + np_], junk[0:np_, dc * 128:(dc + 1) * 128], ident_bf[0:np_, 0:np_])
                for j in range(4):
                    dc = q4 * 4 + j
                    evac_affine(dst_fn(dc), ps[:, j * 128:j * 128 + np_], Ac[:, dc:dc + 1], Bc[:, dc:dc + 1])

        AA.off = 0
        xt_slots = [AA.alloc(F32, [128, 2048]) for _ in range(2)]
        junk = AA.alloc(BF16, [128, 2048])
        import os as _os
        for t in range(int(_os.environ.get("NT", "16"))):
            xt = xt_slots[t % 2]
            dma_sp(xt, din["xw"][t * 128:(t + 1) * 128, :], [], [xt], ("xt", t % 2))
            norm_tile(xt, junk, 128, t, A1c, B1c,
                      lambda dc, t=t: hT[:, t // 8, dc, (t % 8) * 128:(t % 8 + 1) * 128])
        dump("hT", hT_t[:, :], [128, 32768], BF16)
        dump("hTs", hT_t[:, 0:1024], [128, 1024], BF16)
        if stop_after == "B":
            return finish()

        def h_tok(qd):
            return lambda kc: hT[:, qd // 2, kc, (qd % 2) * 512:(qd % 2) * 512 + 512]

        def h_halo(kc):
            return hT[:, 0, kc, 1022:1024]

        def h_tile(t):
            return lambda kc: hT[:, t // 8, kc, (t % 8) * 128:(t % 8 + 1) * 128]

        def proj_fm(sl, m, rhs_fn, w=512, nk=NKC):
            ps = bank()
            for kc in range(nk):
                mm(ps[:, 0:w], sl[:, kc, m * 128:(m + 1) * 128], rhs_fn(kc), kc == 0, kc == nk - 1)
            return ps

        def proj_tm(sl, tiles, dst_fn, post=None):
            for i in range(0, len(tiles), 2):
                ps = bank()
                for u in range(2):
                    lf = h_tile(tiles[i + u])
                    for kc in range(NKC):
                        mm(ps[:, u * 256:(u + 1) * 256], lf(kc), sl[:, kc, :], kc == 0, kc == NKC - 1)
                src = ps[:].rearrange("p (a b) -> p a b", a=2)
                if post is None:
                    evac_copy(dst_fn(i), src)
                else:
                    post(dst_fn(i), src)

        AA.off = 0
        qn = AA.alloc(BF16, [128, 4, 1024])
        kn = AA.alloc(BF16, [128, 4, 2048])
        va = AA.alloc(BF16, [128, 16, 512])
        R_END = AA.off
        yaT = AA.alloc(BF16, [128, 8, 1024])
        Y_END = AA.off
        BT = AA.alloc(F32, [128, 4, 3, 256])
        sq = [AA.alloc(BF16, [128, 512]) for _ in range(2)]
        f32a = [AA.alloc(F32, [128, 512]) for _ in range(2)]
        f32b = [AA.alloc(F32, [128, 512]) for _ in range(2)]
        pT = [AA.alloc(BF16, [128, 256]) for _ in range(3)]
        etmp = [AA.alloc(F32, [128, 256]) for _ in range(2)]
        rinv = AA.alloc(F32, [128, 256])
        e8 = AA.alloc(BF16, [8, 1024])
        nmT = AA.alloc(BF16, [8, 1024])
        qf32 = [AA.alloc(F32, [128, 512]) for _ in range(2)]
        load_const(e8, "e8", cast=True)
        tctr = [0]

        def qknorm(ps, gcol, out, w=512):
            i = tctr[0] % 2
            tctr[0] += 1
            qf = qf32[i][:, 0:w]
            vcopy(qf, ps[:, 0:w])
            act(sq[i][:, 0:w], qf, AF.Square)
            ps2 = bank()
            mm(ps2[:, 0:w], ones_bf, sq[i][:, 0:w], True, True)
            ts(f32a[i][:, 0:w], ps2[:, 0:w], 1.0 / 128, EPS, ALU.mult, ALU.add)
            act(f32a[i][:, 0:w], f32a[i][:, 0:w], AF.Sqrt)
            recip(f32b[i][:, 0:w], f32a[i][:, 0:w])
            stt(out, qf, gcol, f32b[i][:, 0:w], ALU.mult, ALU.mult)

        def gating(hl):
            v3 = kn[:, hl, :].rearrange("p (a b) -> p a b", a=8)
            S.op("dve", lambda e: e.tensor_reduce(km, v3, AX.X, ALU.add), reads=[kn[:, hl, :]], writes=[km])
            ts(km_bf, km, 1.0 / 256, None, ALU.mult)
            psg = psb[7]
            for qt in range(8):
                mm(psg[:, qt * 8:(qt + 1) * 8], qn[:, hl, qt * 128:(qt + 1) * 128], km_bf, True, True)
            tt(gm, psg[:, 0:64], vmask, ALU.add)
            for qt in range(8):
                S.op("dve", lambda e, qt=qt: e.max(mx8[:, qt * 8:(qt + 1) * 8], gm[:, qt * 8:(qt + 1) * 8]),
                     reads=[gm], writes=[mx8[:, qt * 8:(qt + 1) * 8]])
            for qt in range(8):
                ts(nm[:, qt * 8:(qt + 1) * 8], gm[:, qt * 8:(qt + 1) * 8],
                   mx8[:, qt * 8 + 2:qt * 8 + 3], NEG, ALU.is_lt, ALU.mult)
            tt(nm_bf, nm, vmask, ALU.add)
            pst = psb[7][:]
            for hq in range(2):
                for u in range(4):
                    qt = hq * 4 + u
                    tp(pst[0:8, u * 128:(u + 1) * 128], nm_bf[:, qt * 8:(qt + 1) * 8], ident_bf)
                vcopy(nmT[0:8, hq * 512:(hq + 1) * 512], pst[0:8, :])

        lctr = [0]

        def attn_block(hl, h, J, qs, w, btc0, mask_ap, psO, psS, out):
            kts = list(range(2 * (J + 1)))

            def qk(kt):
                n = kt // 2
                psL = psb[4 + lctr[0] % 3]
                pt = pT[lctr[0] % 3]
                lctr[0] += 1
                own = (n == J)
                use_mask = (not own) and (mask_ap is not None)
                mm(psL[:, 0:w], kn[:, hl, kt * 128:(kt + 1) * 128], qs, True, not use_mask)
                if use_mask:
                    mm(psL[:, 0:w], e8[0:8, n * 128:(n + 1) * 128], mask_ap, False, True)
                bti = None
                if own:
                    bti = kt - 2 * J
                elif n == J - 1 and kt % 2 == 1:
                    bti = 2
                et = etmp[lctr[0] % 2]
                if bti is None:
                    ts(et[:, 0:w], psL[:, 0:w], ATT_SCALE, c31[:, h:h + 1], ALU.mult, ALU.add)
                else:
                    stt(et[:, 0:w], psL[:, 0:w], ATT_SCALE, BT[:, hl, bti, btc0:btc0 + w], ALU.mult, ALU.add)
                act(pt[:, 0:w], et[:, 0:w], AF.Exp)
                return pt

            def pv(kt, pt, first, last):
                mm(psO[:, 0:w], va[:, kt, hl * 128:(hl + 1) * 128], pt[:, 0:w], first, last)
                mm(psS[:, 0:w], ones_bf, pt[:, 0:w], first, last)

            prev = None
            for kt in kts:
                pt = qk(kt)
                if prev is not None:
                    pv(prev[0], prev[1], prev[0] == kts[0], False)
                prev = (kt, pt)
            pv(prev[0], prev[1], prev[0] == kts[0], True)
            recip(rinv[:, 0:w], psS[:, 0:w])
            tt(out, psO[:, 0:w], rinv[:, 0:w], ALU.mult)

        def attention(hl, h):
            for jq in range(4):
                attn_block(hl, h, 4 + jq, qn[:, hl, jq * 256:(jq + 1) * 256], 256, 0,
                           nmT[0:8, jq * 256:(jq + 1) * 256], psb[jq % 2], psb[2 + jq % 2],
                           yaT[:, h, jq * 256:(jq + 1) * 256])
            attn_block(hl, h, 3, qnh[:, hl, :], 2, 254, None, psb[0], psb[2], yaTh[:, h, :])

        for g in range(2):
            dma_sp(BT, din["bt"][:, 4 * g:4 * g + 4], [], [BT], "bt")
            for s in range(2):
                sl = slab_load(din["w_in"], 0, NKC, g * 512 + s * 256)
                for m in range(2):
                    for th in range(2):
                        ps = proj_fm(sl, m, h_tok(2 + th))
                        qknorm(ps, qgc[:, 0:1], qn[:, 2 * s + m, th * 512:(th + 1) * 512])
                    ps = proj_fm(sl, m, h_halo, w=2)
                    qknorm(ps, qgc[:, 0:1], qnh[:, 2 * s + m, :], w=2)
            for s in range(2):
                sl = slab_load(din["w_in"], 0, NKC, 1024 + g * 512 + s * 256)
                for m in range(2):
                    for qd in range(4):
                        ps = proj_fm(sl, m, h_tok(qd))
                        qknorm(ps, kgc[:, 0:1], kn[:, 2 * s + m, qd * 512:(qd + 1) * 512])
            for s in range(2):
                sl = slab_load(din["w_in"], 0, NKC, 2048 + g * 512 + s * 256)
                proj_tm(sl, list(range(16)), lambda i, s=s: va[:, i:i + 2, s * 256:(s + 1) * 256])
            if g == 0:
                dump("qn", qn, [128, 4, 1024], BF16)
                dump("kn", kn, [128, 4, 2048], BF16)
                dump("va", va, [128, 16, 512], BF16)
            gating(0)
            if g == 0:
                dump("nmT", nmT, [8, 1024], BF16)
            for hl in range(4):
                attention(hl, 4 * g + hl)
                if hl < 3:
                    gating(hl + 1)
        dump("yaT", yaT, [128, 8, 1024], BF16)
        dump("yaTh", yaTh, [128, 8, 2], BF16)
        if stop_after == "C":
            return finish()

        AA.off = 0
        qrT = AA.alloc(BF16, [128, 2, 1024])
        krT = AA.alloc(BF16, [128, 2, 2048])
        kdT = AA.alloc(BF16, [128, 2, 16, 128])
        vr = AA.alloc(BF16, [128, 16, 256])
        Sb = AA.alloc(BF16, [128, 8, 256])
        sT = AA.alloc(BF16, [128, 8, 128])
        qdT = AA.alloc(BF16, [128, 8, 128])
        assert AA.off <= R_END, AA.off
        AA.off = Y_END
        zt = AA.alloc(BF16, [128, 8, 512])
        sg = AA.alloc(BF16, [128, 8, 256])
        zst = AA.alloc(BF16, [128, 2, 1024])
        sgh = AA.alloc(BF16, [128, 256])
        Sbh = AA.alloc(BF16, [128, 256])
        zth = AA.alloc(BF16, [128, 512])
        ynh = AA.alloc(F32, [128, 256])
        cs = AA.alloc(F32, [128, 2, 512])
        rgb = AA.alloc(F32, [128, 512])
        decTh = AA.alloc(F32, [128, 128])
        qdech = AA.alloc(F32, [128, 128])
        Sst = AA.alloc(F32, [128, 256])
        r32a = [AA.alloc(F32, [128, 512]) for _ in range(2)]
        r32b = [AA.alloc(F32, [128, 512]) for _ in range(2)]
        yn = [AA.alloc(F32, [128, 256]) for _ in range(2)]
        sgtmp = AA.alloc(F32, [128, 2, 256])
        rctr = [0]

        def rotary(ps, out, c0=0, w=512):
            i = rctr[0] % 2
            rctr[0] += 1
            a, b2 = r32a[i], r32b[i]
            tt(a[:, 0:w], ps[:, 0:w], cs[:, 0, c0:c0 + w], ALU.mult)
            tt(b2[0:64, 0:w], ps[64:128, 0:w], cs[64:128, 1, c0:c0 + w], ALU.mult)
            tt(b2[64:128, 0:w], ps[0:64, 0:w], cs[0:64, 1, c0:c0 + w], ALU.mult)
            tt(out, a[:, 0:w], b2[:, 0:w], ALU.add)

        def groupnorm_gate(o, np_, bi, gslice, sgv, ytmp, dst):
            S.op("dve", lambda e: e.bn_stats(bnst[0:np_, bi, :], o), reads=[o], writes=[bnst[0:np_, bi, :]])
            S.op("dve", lambda e: e.bn_aggr(bnmv[0:np_, bi, :], bnst[0:np_, bi, :]),
                 reads=[bnst[0:np_, bi, :]], writes=[bnmv[0:np_, bi, :]])
            act(smallf[0:np_, bi:bi + 1], bnmv[0:np_, bi, 1:2], AF.Sqrt, bias=epsc[0:np_, :], scale=1.0)
            recip(smallf[0:np_, bi:bi + 1], smallf[0:np_, bi:bi + 1])
            ts(ytmp, o, bnmv[0:np_, bi, 0:1], smallf[0:np_, bi:bi + 1], ALU.subtract, ALU.mult)
            tt(ytmp, ytmp, gslice, ALU.mult)
            tt(dst, ytmp, sgv, ALU.mult)

        for rg in range(4):
            slq = slab_load(din["w_in"], 0, NKC, 3072 + rg * 256)
            slk = slab_load(din["w_in"], 0, NKC, 4096 + rg * 256)
            dma_sp(rgb, din["rg_bc"][:, rg * 512:(rg + 1) * 512], [], [rgb], "rgb")
            for qd in range(4):
                dma_sp(cs[:, 0, :], din["cosT"][:, qd * 512:(qd + 1) * 512], [], [cs[:, 0, :]], "cs0")
                dma_sp(cs[:, 1, :], din["sinT"][:, qd * 512:(qd + 1) * 512], [], [cs[:, 1, :]], "cs1")
                for m in range(2):
                    ps = proj_fm(slk, m, h_tok(qd))
                    rotary(ps, krT[:, m, qd * 512:(qd + 1) * 512])
                if qd == 1:
                    for m in range(2):
                        ps = proj_fm(slq, m, h_halo, w=2)
                        rotary(ps, qrTh[:, m, :], c0=510, w=2)
                if qd >= 2:
                    for m in range(2):
                        ps = proj_fm(slq, m, h_tok(qd))
                        rotary(ps, qrT[:, m, (qd - 2) * 512:(qd - 1) * 512])
            if rg == 0:
                dump("qrT", qrT, [128, 2, 1024], BF16)
                dump("krT", krT, [128, 2, 2048], BF16)
            for m in range(2):
                h = 2 * rg + m
                for qq in range(4):
                    pst = bank()
                    for u in range(4):
                        t = qq * 4 + u
                        tp(pst[:, u * 128:(u + 1) * 128], krT[:, m, t * 128:(t + 1) * 128], ident_bf)
                    dst = kdT[:, m, qq * 4:(qq + 1) * 4, :]
                    src = pst.rearrange("p (a b) -> p a b", a=4)
                    ts(dst, src, kdec[:, h:h + 1], None, ALU.mult)
            for m in range(2):
                h = 2 * rg + m
                dma_sp(decTh, din["decT"][:, h, :], [], [decTh], "decTh")
                dma_sp(qdech, din["qdec"][:, h, :], [], [qdech], "qdech")
                slv = slab_load(din["w_in"], 0, NKC, 5120 + h * 256)
                proj_tm(slv, list(range(16)), lambda i: vr[:, i:i + 2, :])
                slg = slab_load(din["w_in"], 0, NKC, 7168 + h * 256)
                def _silu_post(d, s_):
                    vcopy(sgtmp, s_)
                    act(d, sgtmp, AF.Silu)
                proj_tm(slg, list(range(8, 16)), lambda i: sg[:, i:i + 2, :], post=_silu_post)
                psh = bank()
                for kc in range(NKC):
                    mm(psh[0:2, 0:256], h_halo(kc), slg[:, kc, :], kc == 0, kc == NKC - 1)
                vcopy(ynh[0:2, :], psh[0:2, 0:256])
                act(sgh[0:2, :], ynh[0:2, :], AF.Silu)
                for n in range(15):
                    o = bank()[:, 0:256]
                    mm(o, kdT[:, m, n, :], vr[:, n, :], True, True)
                    if n == 0:
                        vcopy(Sst, o)
                    else:
                        if n == 7:
                            acopy(Sbh, Sst)
                        if n == 8:
                            ts(Sst, Sst, flag[:, 0:1], None, ALU.mult)
                        if n >= 8:
                            acopy(Sb[:, n - 8, :], Sst)
                        stt(Sst, Sst, cd[h], o, ALU.mult, ALU.add)
                acopy(Sb[:, 7, :], Sst)
                for c4 in range(2):
                    pss = bank()
                    for u in range(4):
                        c = c4 * 4 + u
                        mm(pss[:, u * 128:(u + 1) * 128], krT[:, m, (8 + c) * 128:(9 + c) * 128],
                           qrT[:, m, c * 128:(c + 1) * 128], True, True)
                    for u in range(4):
                        c = c4 * 4 + u
                        tt(sT[:, c, :], pss[:, u * 128:(u + 1) * 128], decTh, ALU.mult)
                for c in range(8):
                    tt(qdT[:, c, :], qrT[:, m, c * 128:(c + 1) * 128], qdech, ALU.mult)
                for c in range(8):
                    o = bank()[:, 0:256]
                    mm(o, sT[:, c, :], vr[:, 8 + c, :], True, False)
                    mm(o, qdT[:, c, :], Sb[:, c, :], False, True)
                    groupnorm_gate(o, 128, c % 2, rgb[:, m * 256:(m + 1) * 256], sg[:, c, :], yn[c % 2],
                                   zt[:, c, m * 256:(m + 1) * 256])
                pss = bank()
                mm(pss[:, 0:2], krT[:, m, 7 * 128:8 * 128], qrTh[:, m, :], True, True)
                tt(sTh, pss[:, 0:2], decTh[:, 126:128], ALU.mult)
                tt(qdTh, qrTh[:, m, :], qdech[:, 126:128], ALU.mult)
                psy = bank()
                mm(psy[0:2, 0:256], sTh, vr[:, 7, :], True, False)
                mm(psy[0:2, 0:256], qdTh, Sbh, False, True)
                groupnorm_gate(psy[0:2, 0:256], 2, 2, rgb[0:2, m * 256:(m + 1) * 256], sgh[0:2, :],
                               ynh[0:2, :], zth[0:2, m * 256:(m + 1) * 256])
            if rg == 0:
                dump("zt", zt, [128, 8, 512], BF16)
            for fc in range(4):
                for hc in range(2):
                    pst = bank()
                    for u in range(4):
                        c = hc * 4 + u
                        tp(pst[:, u * 128:(u + 1) * 128], zt[:, c, fc * 128:(fc + 1) * 128], ident_bf)
                    evac_copy(zst[:, fc % 2, hc * 512:(hc + 1) * 512], pst)
                if fc % 2 == 1:
                    a0 = rg * 4 + fc - 1
                    dma_sp(zTs[a0:a0 + 2].rearrange("a p t -> p a t"), zst, [zst], ["zTs"], "zst")
            psth = bank()
            for fc in range(4):
                tp(psth[:, fc * 2:fc * 2 + 2], zth[0:2, fc * 128:(fc + 1) * 128], ident_bf[0:2, 0:2])
            vcopy(zTh[:, rg * 4:(rg + 1) * 4, :], psth[:, 0:8].rearrange("p (a b) -> p a b", a=4))
        dump("zTh", zTh, [128, 16, 2], BF16)
        if stop_after == "D":
            return finish()

        zT = carve(hT_t, 0, BF16, [128, 16, 1024])
        hTh = CA.alloc(BF16, [128, 16, 2])
        vcopy(hTh, hT[:, 0, :, 1022:1024])
        dma_sp(zT, zTs.rearrange("a p t -> p a t"), ["zTs"], [zT], "zTl")
        AA.off = Y_END
        mT = AA.alloc(BF16, [128, 16, 1024])
        sga = [AA.alloc(F32, [128, 512]) for _ in range(2)]
        sgb = [AA.alloc(F32, [128, 512]) for _ in range(2)]
        ectr = [0]

        def merge_group(slga, slgb, sla, slr, m, rhs_h, rhs_ya, rhs_z, w, dst):
            i = ectr[0] % 2
            ectr[0] += 1
            a, b2 = sga[i][:, 0:w], sgb[i][:, 0:w]
            pga = proj_fm(slga, m, rhs_h, w=w)
            vcopy(a, pga[:, 0:w])
            act(a, a, AF.Sigmoid)
            pgb = proj_fm(slgb, m, rhs_h, w=w)
            vcopy(b2, pgb[:, 0:w])
            act(b2, b2, AF.Sigmoid)
            pa = proj_fm(sla, m, rhs_ya, w=w, nk=8)
            tt(a, pa[:, 0:w], a, ALU.mult)
            pr = proj_fm(slr, m, rhs_z, w=w)
            tt(b2, pr[:, 0:w], b2, ALU.mult)
            tt(dst, a, b2, ALU.add)

        for dg in range(8):
            sla = slab_load(din["w_attn_br"], 0, 8, dg * 256)
            slr = slab_load(din["w_ret_br"], 0, NKC, dg * 256)
            slga = slab_load(din["w_in"], 0, NKC, 9216 + dg * 256)
            slgb = slab_load(din["w_in"], 0, NKC, 11264 + dg * 256)
            for m in range(2):
                dc = dg * 2 + m
                for th in range(2):
                    tsl = slice(th * 512, (th + 1) * 512)
                    merge_group(slga, slgb, sla, slr, m, h_tok(2 + th),
                                lambda kc, tsl=tsl: yaT[:, kc, tsl], lambda kc, tsl=tsl: zT[:, kc, tsl],
                                512, mT[:, dc, tsl])
                merge_group(slga, slgb, sla, slr, m, lambda kc: hTh[:, kc, :],
                            lambda kc: yaTh[:, kc, :], lambda kc: zTh[:, kc, :], 2, mTh[:, dc, :])
        dump("mT", mT, [128, 16, 1024], BF16)
        dump("mTh", mTh, [128, 16, 2], BF16)
        if stop_after == "E":
            return finish()

        AA.off = 0
        g1bc = AA.alloc(F32, [128, 2048])
        btmp = AA.alloc(F32, [128, 2048])
        xin = [AA.alloc(F32, [128, 8, 256]) for _ in range(2)]
        xh = AA.alloc(F32, [128, 2048])
        assert AA.off <= Y_END
        dma_sp(btmp, din["b_g1"], [], [btmp], "btmp")
        dma_sp(xh[0:2, :], din["xw"][1022:1024, :], [], [xh[0:2, :]], "xh")
        s_rep = AA.alloc(BF16, [128, 16, 128])
        assert AA.off <= Y_END
        build_s_rep(s_rep)
        mod_row_section(2, g1bc, s_rep)
        tt(g1bc, g1bc, btmp, ALU.add)
        xown = din["xw"][1024:2048, :]
        for dg in range(8):
            dsl = slice(dg * 256, (dg + 1) * 256)
            sl = slab_load(din["w_o"], 0, NKC, dg * 256)
            xi = xin[dg % 2]
            dma_sp(xi, xown[:, dsl].rearrange("(t p) n -> p t n", p=128), [], [xi], ("xin", dg % 2))
            for t2 in range(4):
                ps = bank()
                for u in range(2):
                    t = t2 * 2 + u
                    for kc in range(NKC):
                        mm(ps[:, u * 256:(u + 1) * 256], mT[:, kc, t * 128:(t + 1) * 128], sl[:, kc, :],
                           kc == 0, kc == NKC - 1)
                for u in range(2):
                    tt(ps[:, u * 256:(u + 1) * 256], ps[:, u * 256:(u + 1) * 256], g1bc[:, dsl], ALU.mult)
                src = ps[:].rearrange("p (a b) -> p a b", a=2)
                tt(xi[:, t2 * 2:t2 * 2 + 2, :], xi[:, t2 * 2:t2 * 2 + 2, :], src, ALU.add)
            ps = bank()
            for kc in range(NKC):
                mm(ps[0:2, 0:256], mTh[:, kc, :], sl[:, kc, :], kc == 0, kc == NKC - 1)
            tt(ps[0:2, 0:256], ps[0:2, 0:256], g1bc[0:2, dsl], ALU.mult)
            tt(xh[0:2, dsl], xh[0:2, dsl], ps[0:2, 0:256], ALU.add)
            dma_sp(x1s[:, dsl].rearrange("(t p) n -> p t n", p=128), xi, [xi], [("x1s", dg)], ("x1w", dg % 2))
        if "x1" in debug:
            xd = AA.alloc(F32, [128, 2048])
            dma_sp(xd, x1s[0:128, :], X1KEYS, [xd], "xd")
            dump("x1", xd, [128, 2048], F32)
            dump("xh", xh[0:2, :], [2, 2048], F32)
        if stop_after == "F":
            return finish()

        mod_col_section(4, 3)
        mod_col_section(3, 2)
        ts(A2c, modc[:, 3, :], 1.0, None, ALU.add)
        tt(A2c, A2c, g2c, ALU.mult)
        B2c = modc[:, 2, :]
        h2T = carve(hT_t, 32768, BF16, [128, NKC, 1024])
        HA.off = 0
        xt2 = [HA.alloc(F32, [128, 2048]) for _ in range(2)]
        junk2 = HA.alloc(BF16, [128, 2048])
        assert HA.off <= 32768
        for t in range(8):
            xt = xt2[t % 2]
            dma_sp(xt, x1s[t * 128:(t + 1) * 128, :], X1KEYS, [xt], ("xt2", t % 2))
            norm_tile(xt, junk2, 128, t, A2c, B2c, lambda dc, t=t: h2T[:, dc, t * 128:(t + 1) * 128])
        norm_tile(xh[0:2, :], junk2, 2, 16, A2c, B2c, lambda dc: h2Th[:, dc, :])
        dump("h2T", h2T, [128, NKC, 1024], BF16)
        dump("h2Th", h2Th, [128, NKC, 2], BF16)
        if stop_after == "G":
            return finish()

        actT = carve(A_t, 0, BF16, [128, NFC, 1024])
        HA.off = 0
        cwt = HA.alloc(F32, [128, 88, 3])
        cbt = HA.alloc(F32, [128, 88])
        uv = [HA.alloc(F32, [128, 1024]) for _ in range(2)]
        ug = [HA.alloc(F32, [128, 1024]) for _ in range(2)]
        sgt = [HA.alloc(F32, [128, 1024]) for _ in range(2)]
        assert HA.off <= 32768, HA.off
        dma_sp(cwt, din["cw"], [], [cwt], "cwt")
        dma_sp(cbt, din["cb"], [], [cbt], "cbt")

        def conv(dst, psl, ph, ch):
            w0, w1, w2 = cwt[:, ch, 0:1], cwt[:, ch, 1:2], cwt[:, ch, 2:3]
            for th in range(2):
                o = dst[:, th * 512:(th + 1) * 512]
                ts(o, psl[th], w2, cbt[:, ch:ch + 1], ALU.mult, ALU.add)
            for th in range(2):
                b0 = th * 512
                stt(dst[:, b0 + 1:b0 + 512], psl[th][:, 0:511], w1, dst[:, b0 + 1:b0 + 512], ALU.mult, ALU.add)
                stt(dst[:, b0 + 2:b0 + 512], psl[th][:, 0:510], w0, dst[:, b0 + 2:b0 + 512], ALU.mult, ALU.add)
            stt(dst[:, 512:513], psl[0][:, 511:512], w1, dst[:, 512:513], ALU.mult, ALU.add)
            stt(dst[:, 512:514], psl[0][:, 510:512], w0, dst[:, 512:514], ALU.mult, ALU.add)
            stt(dst[:, 0:1], ph[:, 1:2], w1, dst[:, 0:1], ALU.mult, ALU.add)
            stt(dst[:, 0:2], ph[:, 0:2], w0, dst[:, 0:2], ALU.mult, ALU.add)

        def up_chunk(sl, m, ch, dst, hi):
            ps2 = [proj_fm(sl, m, lambda kc, th=th: h2T[:, kc, th * 512:(th + 1) * 512]) for th in range(2)]
            psh = proj_fm(sl, m, lambda kc: h2Th[:, kc, :], w=2)
            ts(Phs[:, hi, :], psh[:, 0:2], flag[:, 0:1], None, ALU.mult)
            conv(dst, ps2, Phs[:, hi, :], ch)

        fctr = [0]
        for fp in range(22):
            slv = slab_load(din["w_up"], 0, NKC, fp * 256)
            slg = slab_load(din["w_up"], 0, NKC, FFN + fp * 256)
            for m in range(2):
                fc = fp * 2 + m
                i = fctr[0] % 2
                fctr[0] += 1
                up_chunk(slv, m, fc, uv[i], 0)
                up_chunk(slg, m, 44 + fc, ug[i], 1)
                act(sgt[i], ug[i], AF.Silu)
                tt(actT[:, fc, :], uv[i], sgt[i], ALU.mult)
        dump("actT", actT, [128, NFC, 1024], BF16)
        if stop_after == "H":
            return finish()

        HA.off = 0
        g2bc = HA.alloc(F32, [128, 2048])
        btmp2 = HA.alloc(F32, [128, 2048])
        xo = [HA.alloc(F32, [128, 8, 256]) for _ in range(2)]
        assert HA.off <= 32768
        dma_sp(btmp2, din["b_g2"], [], [btmp2], "btmp2")
        s_rep2 = carve(A_t, 90112, BF16, [128, 16, 128])
        build_s_rep(s_rep2)
        mod_row_section(5, g2bc, s_rep2)
        tt(g2bc, g2bc, btmp2, ALU.add)
        ykeys = []
        for dg in range(8):
            dsl = slice(dg * 256, (dg + 1) * 256)
            xi = xo[dg % 2]
            dma_sp(xi, x1s[:, dsl].rearrange("(t p) n -> p t n", p=128), X1KEYS, [xi], ("xo", dg % 2))
            for (k0, nk) in [(0, 16), (16, 16), (32, 12)]:
                sl = slab_load(din["w_down"], k0 * 128, nk, dg * 256)
                for t in range(8):
                    o = psb[t][:, 0:256]
                    for kk in range(nk):
                        kc = k0 + kk
                        mm(o, actT[:, kc, t * 128:(t + 1) * 128], sl[:, kk, :], kc == 0, kc == NFC - 1)
            for t in range(8):
                ps = psb[t]
                tt(ps[:, 0:256], ps[:, 0:256], g2bc[:, dsl], ALU.mult)
                tt(xi[:, t, :], xi[:, t, :], ps[:, 0:256], ALU.add)
            dma_sp(y[:, dsl].rearrange("(t p) n -> p t n", p=128), xi, [xi], [("y", dg)], ("yw", dg))
            ykeys.append(("yw", dg))
        return finish(extra=ykeys)


_CACHE = {}


def input_names(nc):
    out = []
    for a in nc.m.functions[0].allocations:
        if isinstance(a, mybir.MemoryLocationSet) and a.kind == "ExternalInput":
            out.append(a.memorylocations[0].name)
    return out


def kernel(**inputs):
    inputs = {k: np.asarray(v) for k, v in inputs.items()}
    if "nc" not in _CACHE:
        _CACHE["nc"] = build_program()[0]
        _CACHE["names"] = input_names(_CACHE["nc"])
    nc = _CACHE["nc"]
    names = set(_CACHE["names"])
    in_maps = [{k: v for k, v in make_core_inputs(inputs, c).items() if k in names} for c in range(8)]
    res = run_bass_kernel_spmd(nc, in_maps, core_ids=list(range(8)))
    out = np.empty((4, 2048, D), np.float32)
    for c in range(8):
        out[c // 2, (c % 2) * 1024:(c % 2 + 1) * 1024, :] = res.results[c]["y"]
    return out
```

```python
import contextlib
import math

import numpy as np
import concourse.bass as bass
import concourse.mybir as mybir
from concourse.bass_utils import run_bass_kernel_spmd

F32 = mybir.dt.float32
BF16 = mybir.dt.bfloat16
ALU = mybir.AluOpType
AF = mybir.ActivationFunctionType
AX = mybir.AxisListType

D = 2048
OWN = 1024
WIN = 2048
NKC = 16
NEG = -30000.0
EPS = 1e-6
FFN = 5632
NFC = 44
ATT_SCALE = 128 ** -0.5
ENGS = ("pe", "act", "dve", "pool", "sp")
GRAN = 512


def _esz(dt):
    return 4 if dt == F32 else 2


class Sched:
    def __init__(self, nc):
        self.nc = nc
        self.ops = {e: [] for e in ENGS}
        self.last_w = {}
        self.readers = {}
        self.dma_cnt = {}

    @staticmethod
    def keys(x):
        if isinstance(x, (str, tuple)):
            return [x]
        ap = x.ap
        esz = _esz(x.dtype)
        pstride = ap[0][0]
        off = x.offset % pstride if pstride > 0 else x.offset
        ext = 1
        for (st, cnt) in ap[1:]:
            ext += abs(st) * (cnt - 1)
        lo = off * esz
        hi = (off + ext) * esz
        name = x.tensor.name
        if name.startswith("ps"):
            return [(name, 0)]
        return [(name, g) for g in range(lo // GRAN, (hi - 1) // GRAN + 1)]

    def op(self, eng, fn, reads=(), writes=(), dma=None):
        idx = len(self.ops[eng])
        me = (eng, idx)
        deps = set()
        rk = [k for r in reads for k in self.keys(r)]
        wk = [k for w in writes for k in self.keys(w)]
        for k in rk:
            w = self.last_w.get(k)
            if w is not None:
                deps.add(w)
        for k in wk:
            w = self.last_w.get(k)
            if w is not None:
                deps.add(w)
            for r in self.readers.get(k, {}).items():
                deps.add(r)
        deps.discard(me)
        rec = dict(fn=fn, deps=deps, dma=dma, signal=False, cnt=None)
        if dma is not None:
            self.dma_cnt[dma] = self.dma_cnt.get(dma, 0) + 1
            rec["cnt"] = 16 * self.dma_cnt[dma]
            rec["signal"] = True
        self.ops[eng].append(rec)
        for k in rk:
            self.readers.setdefault(k, {})[eng] = idx
        for k in wk:
            self.last_w[k] = me
            self.readers[k] = {}
        return me

    def emit(self, final_wait=()):
        nc = self.nc
        ops = self.ops
        for e in ENGS:
            for rec in ops[e]:
                nd = set()
                for (de, di) in rec["deps"]:
                    drec = ops[de][di]
                    if de == e and drec["dma"] is None and e in ("pe", "sp"):
                        continue
                    nd.add((de, di))
                rec["deps"] = nd
                for (de, di) in nd:
                    if ops[de][di]["dma"] is None:
                        ops[de][di]["signal"] = True
        for e in ENGS:
            c = 0
            for rec in ops[e]:
                if rec["dma"] is None and rec["signal"]:
                    c += 1
                    rec["cnt"] = c
        with contextlib.ExitStack() as st:
            esem = {e: st.enter_context(nc.semaphore("s_" + e)) for e in ENGS}
            dsem = {k: st.enter_context(nc.semaphore("d_%d" % i))
                    for i, k in enumerate(self.dma_cnt)}
            block = st.enter_context(nc.Block())

            def run(e):
                def body(engobj):
                    waited = {}
                    for rec in ops[e]:
                        need = {}
                        for (de, di) in rec["deps"]:
                            drec = ops[de][di]
                            s = ("d", drec["dma"]) if drec["dma"] is not None else ("e", de)
                            need[s] = max(need.get(s, 0), drec["cnt"])
                        for s, v in need.items():
                            if waited.get(s, 0) >= v:
                                continue
                            waited[s] = v
                            sem = dsem[s[1]] if s[0] == "d" else esem[s[1]]
                            engobj.wait_ge(sem, v)
                        ins = rec["fn"](engobj)
                        if rec["dma"] is not None:
                            ins.then_inc(dsem[rec["dma"]], 16)
                        elif rec["signal"]:
                            ins.then_inc(esem[e], 1)
                    if e == "sp":
                        for k in final_wait:
                            engobj.wait_ge(dsem[k], 16 * self.dma_cnt[k])
                return body

            block.tensor(run("pe"))
            block.scalar(run("act"))
            block.vector(run("dve"))
            block.gpsimd(run("pool"))
            block.sync(run("sp"))


def _t5_bucket(dist):
    n = np.maximum(dist, 0)
    max_exact = 16
    nf = np.maximum(n, 1).astype(np.float32)
    large = max_exact + (np.log(nf / max_exact) / math.log(128 / max_exact) * (32 - max_exact)).astype(np.int32)
    large = np.minimum(large, 31)
    return np.where(n < max_exact, n, large)


def _const_tables(half):
    out = {}
    k = np.arange(128)[:, None]
    q = np.arange(256)[None, :]
    dists = [q - k, q - k - 128, q - k + 128]
    out["bt_bucket"] = [_t5_bucket(d) for d in dists]
    out["bt_valid"] = [d >= 0 for d in dists]
    vm = np.full((8, 8), NEG, np.float32)
    for qt in range(8):
        j = qt // 2
        for n in range(8):
            ok = (n < 4 + j) and (n >= 4 or half == 1)
            if ok:
                vm[qt, n] = 0.0
    out["vmask"] = np.ascontiguousarray(np.broadcast_to(vm.reshape(1, 64), (128, 64))).astype(np.float32)
    e8 = np.zeros((8, 8, 128), np.float32)
    for n in range(8):
        e8[n, n, :] = 1.0
    out["e8"] = e8.reshape(8, 1024)
    pos = (np.arange(WIN) - 1024 + half * 1024).astype(np.float32)
    freqs = np.power(np.float32(10000.0), -np.arange(64, dtype=np.float32) / 64).astype(np.float32)
    ang = (pos[None, :] * freqs[:, None]).astype(np.float32)
    cos = np.cos(ang).astype(np.float32)
    sin = np.sin(ang).astype(np.float32)
    out["cosT"] = np.concatenate([cos, cos], 0)
    out["sinT"] = np.concatenate([sin, -sin], 0)
    hh = np.arange(8, dtype=np.float32)
    log_decay = np.log(1.0 - np.power(2.0, -5.0 - hh)).astype(np.float32)
    i = np.arange(128, dtype=np.float32)
    diff = i[:, None] - i[None, :]
    inner = np.where(diff >= 0, np.exp(log_decay[:, None, None] * np.maximum(diff, 0.0)), 0.0)
    sc = np.float32(128 ** -0.5)
    out["decT"] = np.ascontiguousarray(inner.transpose(2, 0, 1) * sc).astype(np.float32)
    qd = np.exp(log_decay[:, None] * (i + 1.0)).astype(np.float32)
    out["qdec"] = np.ascontiguousarray(np.broadcast_to(qd[None], (128, 8, 128))).astype(np.float32)
    kd = np.exp(log_decay[:, None] * (127.0 - i)).astype(np.float32) * sc
    out["kdec"] = np.ascontiguousarray(kd.T).astype(np.float32)
    out["chunk_decay"] = [float(np.exp(np.float32(log_decay[h] * 128.0))) for h in range(8)]
    out["flag"] = np.full((128, 1), float(half), np.float32)
    return out


def _col(v, n):
    return np.ascontiguousarray(np.asarray(v, np.float32).reshape(n, 128).T)


def _bc(v):
    v = np.asarray(v, np.float32).reshape(1, -1)
    return np.ascontiguousarray(np.broadcast_to(v, (128, v.shape[1])))


def make_core_inputs(inp, core):
    b, half = core // 2, core % 2
    ct = _const_tables(half)
    x = np.asarray(inp["x"], np.float32)
    m = {}
    if half == 1:
        m["xw"] = np.ascontiguousarray(x[b])
    else:
        m["xw"] = np.ascontiguousarray(np.concatenate([np.zeros((1024, D), np.float32), x[b, :1024]], 0))
    m["c_col"] = _col(inp["c"][b], 16)
    m["w_ada"] = np.ascontiguousarray(inp["w_ada"][0])
    bada = np.asarray(inp["b_ada"][0], np.float32)
    m["b_col"] = _col(bada, 96)
    m["b_g1"] = _bc(bada[4096:6144])
    m["b_g2"] = _bc(bada[10240:12288])
    m["g1_col"] = _col(inp["norm1_g"][0], 16)
    m["g2_col"] = _col(inp["norm2_g"][0], 16)
    m["w_in"] = np.ascontiguousarray(inp["w_in"][0])
    m["qg_col"] = _col(inp["q_norm_g"][0], 1)
    m["kg_col"] = _col(inp["k_norm_g"][0], 1)
    rb = np.asarray(inp["rel_bias"], np.float32)
    bt = np.empty((128, 8, 3, 256), np.float32)
    for j in range(3):
        g = rb[ct["bt_bucket"][j]]
        g = np.where(ct["bt_valid"][j][:, :, None], g, np.float32(NEG))
        bt[:, :, j, :] = g.transpose(0, 2, 1)
    m["bt"] = bt
    m["c31"] = _bc(rb[31])
    m["vmask"] = ct["vmask"]
    m["e8"] = ct["e8"]
    m["rg_bc"] = _bc(inp["ret_norm_g"][0])
    m["cosT"] = ct["cosT"]
    m["sinT"] = ct["sinT"]
    m["decT"] = ct["decT"]
    m["qdec"] = ct["qdec"]
    m["kdec"] = ct["kdec"]
    m["flag"] = ct["flag"]
    m["w_attn_br"] = np.ascontiguousarray(inp["w_attn_br"][0])
    m["w_ret_br"] = np.ascontiguousarray(inp["w_ret_br"][0])
    m["w_o"] = np.ascontiguousarray(inp["w_o"][0])
    m["w_up"] = np.ascontiguousarray(inp["w_up"][0])
    cw = np.asarray(inp["conv_w"][0], np.float32)
    m["cw"] = np.ascontiguousarray(cw.reshape(3, 88, 128).transpose(2, 1, 0))
    m["cb"] = _col(inp["conv_b"][0], 88)
    m["w_down"] = np.ascontiguousarray(inp["w_down"][0])
    m["ident"] = np.eye(128, dtype=np.float32)
    return m


INPUT_SHAPES = {
    "xw": [2048, 2048], "c_col": [128, 16], "w_ada": [2048, 12288], "b_col": [128, 96],
    "b_g1": [128, 2048], "b_g2": [128, 2048], "g1_col": [128, 16], "g2_col": [128, 16],
    "w_in": [2048, 13312], "qg_col": [128, 1], "kg_col": [128, 1], "bt": [128, 8, 3, 256],
    "c31": [128, 8], "vmask": [128, 64], "e8": [8, 1024], "rg_bc": [128, 2048],
    "cosT": [128, 2048], "sinT": [128, 2048], "decT": [128, 8, 128], "qdec": [128, 8, 128],
    "kdec": [128, 8], "flag": [128, 1], "w_attn_br": [1024, 2048], "w_ret_br": [2048, 2048],
    "w_o": [2048, 2048], "w_up": [2048, 11264], "cw": [128, 88, 3], "cb": [128, 88],
    "w_down": [5632, 2048], "ident": [128, 128],
}


def build_program(stop_after=None, debug=()):
    nc = bass.Bass("TRN2", target_bir_lowering=False)
    class _LazyIn(dict):
        def __missing__(self, k):
            self[k] = nc.dram_tensor(k, INPUT_SHAPES[k], F32, kind="ExternalInput").ap()
            return self[k]
    din = _LazyIn()
    y = nc.dram_tensor("y", [OWN, D], F32, kind="ExternalOutput").ap()
    x1s = nc.dram_tensor("x1s", [OWN, D], F32, kind="Internal").ap()
    zTs = nc.dram_tensor("zTs", [16, 128, OWN], BF16, kind="Internal").ap()
    dbg_out = {}
    cd = _const_tables(1)["chunk_decay"]
    X1KEYS = [("x1s", d) for d in range(8)]

    with contextlib.ExitStack() as st:
        slab_t = st.enter_context(nc.sbuf_tensor("slab", [128, 4, NKC, 256], BF16))
        hT_t = st.enter_context(nc.sbuf_tensor("hT", [128, 32768], BF16))
        A_t = st.enter_context(nc.sbuf_tensor("A", [128, 49152], BF16))
        C_t = st.enter_context(nc.sbuf_tensor("C", [128, 4096], BF16))
        psb = [st.enter_context(nc.psum_tensor("ps%d" % i, [128, 512], F32)) for i in range(8)]
        S = Sched(nc)

        def carve(t, boff, dt, shape):
            n = 1
            for s in shape[1:]:
                n *= s
            nb = n * _esz(dt)
            v = t[:, boff // 2:(boff + nb) // 2]
            if dt == F32:
                v = v.bitcast(F32)
            if len(shape) == 3:
                v = v.rearrange("p (a b) -> p a b", a=shape[1])
            elif len(shape) == 4:
                v = v.rearrange("p (a b c) -> p a b c", a=shape[1], b=shape[2])
            if shape[0] != 128:
                v = v[0:shape[0]]
            return v

        class Arena:
            def __init__(self, t, size):
                self.t, self.size, self.off = t, size, 0

            def alloc(self, dt, shape):
                self.off = (self.off + 63) // 64 * 64
                v = carve(self.t, self.off, dt, shape)
                n = _esz(dt)
                for s in shape[1:]:
                    n *= s
                self.off += n
                assert self.off <= self.size, (self.off, self.size)
                return v

        CA = Arena(C_t, 8192)
        AA = Arena(A_t, 98304)
        HA = Arena(hT_t, 65536)

        ident32 = CA.alloc(F32, [128, 128])
        ident_bf = CA.alloc(BF16, [128, 128])
        ones_bf = CA.alloc(BF16, [128, 128])
        ccol = CA.alloc(F32, [128, 16])
        s_bf = CA.alloc(BF16, [128, 16])
        s_bf2 = CA.alloc(BF16, [128, 16, 2])
        bcol = CA.alloc(F32, [128, 96])
        modc = CA.alloc(F32, [128, 4, 16])
        g1c = CA.alloc(F32, [128, 16])
        g2c = CA.alloc(F32, [128, 16])
        A1c = CA.alloc(F32, [128, 16])
        A2c = CA.alloc(F32, [128, 16])
        qgc = CA.alloc(F32, [128, 1])
        kgc = CA.alloc(F32, [128, 1])
        c31 = CA.alloc(F32, [128, 8])
        vmask = CA.alloc(F32, [128, 64])
        kdec = CA.alloc(F32, [128, 8])
        flag = CA.alloc(F32, [128, 1])
        epsc = CA.alloc(F32, [128, 1])
        ssq = CA.alloc(F32, [128, 17])
        rstd = CA.alloc(F32, [128, 17])
        smallf = CA.alloc(F32, [128, 8])
        km = CA.alloc(F32, [128, 8])
        km_bf = CA.alloc(BF16, [128, 8])
        gm = CA.alloc(F32, [128, 64])
        mx8 = CA.alloc(F32, [128, 64])
        nm = CA.alloc(F32, [128, 64])
        nm_bf = CA.alloc(BF16, [128, 64])
        bnst = CA.alloc(F32, [128, 3, 6])
        bnmv = CA.alloc(F32, [128, 3, 2])
        qnh = CA.alloc(BF16, [128, 4, 2])
        yaTh = CA.alloc(BF16, [128, 8, 2])
        qrTh = CA.alloc(BF16, [128, 2, 2])
        sTh = CA.alloc(BF16, [128, 2])
        qdTh = CA.alloc(BF16, [128, 2])
        zTh = CA.alloc(BF16, [128, 16, 2])
        mTh = CA.alloc(BF16, [128, 16, 2])
        h2Th = CA.alloc(BF16, [128, 16, 2])
        Phs = CA.alloc(F32, [128, 2, 2])

        def dma_sp(out, in_, reads, writes, key):
            S.op("sp", lambda e: e.dma_start(out=out, in_=in_), reads=reads, writes=writes, dma=key)

        def dma_pool(out, in_, reads, writes, key):
            S.op("pool", lambda e: e.dma_start(out=out, in_=in_), reads=reads, writes=writes, dma=key)

        def load_const(dst, name, cast=False):
            if cast:
                dma_pool(dst, din[name], [], [dst], "c_" + name + "_c")
            else:
                dma_sp(dst, din[name], [], [dst], "c_" + name)

        def mm(out, lhsT, rhs, start, stop):
            S.op("pe", lambda e: e.matmul(out, lhsT, rhs, start=start, stop=stop),
                 reads=[lhsT, rhs], writes=[out])

        def tp(out, in_, ident):
            S.op("pe", lambda e: e.matmul(out, in_, ident, start=True, stop=True), reads=[in_, ident], writes=[out])

        def act(out, in_, func, bias=None, scale=None, accum=None):
            kw = {}
            r = [in_]
            w = [out]
            if bias is not None:
                kw["bias"] = bias
                if not isinstance(bias, float):
                    r.append(bias)
            if scale is not None:
                kw["scale"] = scale
                if not isinstance(scale, float):
                    r.append(scale)
            if accum is not None:
                kw["accum_out"] = accum
                w.append(accum)
            S.op("act", lambda e: e.activation(out, in_, func, **kw), reads=r, writes=w)

        def ts(out, in0, s1, s2, op0, op1=None):
            r = [in0]
            if not isinstance(s1, float):
                r.append(s1)
            if s2 is not None and not isinstance(s2, float):
                r.append(s2)
            if op1 is None:
                S.op("dve", lambda e: e.tensor_scalar(out, in0, s1, None, op0), reads=r, writes=[out])
            else:
                S.op("dve", lambda e: e.tensor_scalar(out, in0, s1, s2, op0, op1), reads=r, writes=[out])

        def tt(out, in0, in1, op):
            S.op("dve", lambda e: e.tensor_tensor(out, in0, in1, op), reads=[in0, in1], writes=[out])

        def stt(out, in0, sc, in1, op0, op1):
            r = [in0, in1]
            if not isinstance(sc, float):
                r.append(sc)
            S.op("dve", lambda e: e.scalar_tensor_tensor(out, in0, sc, in1, op0, op1), reads=r, writes=[out])

        def recip(out, in_):
            S.op("dve", lambda e: e.reciprocal(out, in_), reads=[in_], writes=[out])

        def memset(ap, v):
            S.op("dve", lambda e: e.memset(ap, v), reads=[], writes=[ap])

        def acopy(out, in_):
            S.op("act", lambda e: e.copy(out, in_), reads=[in_], writes=[out])

        def vcopy(out, in_):
            S.op("dve", lambda e: e.tensor_copy(out, in_), reads=[in_], writes=[out])

        ev_tog = [0]

        def evac_affine(out, in_, sc, bi):
            ts(out, in_, sc, bi, ALU.mult, ALU.add)

        def evac_copy(out, in_):
            vcopy(out, in_)

        bank_ctr = [0]

        def bank():
            b = bank_ctr[0] % 8
            bank_ctr[0] += 1
            return psb[b][:]

        slab_ctr = [0]

        def slab_load(w, r0, nk, c0, ncols=256):
            i = slab_ctr[0] % 4
            slab_ctr[0] += 1
            dst = slab_t[:, i, 0:nk, 0:ncols]
            src = w[r0:r0 + nk * 128, c0:c0 + ncols].rearrange("(kc p) n -> p kc n", p=128)
            dma_pool(dst, src, [], [slab_t[:, i, :, :]], ("slab", i))
            return slab_t[:, i]

        def dump(name, ap, shape, dt):
            if name not in debug:
                return
            o = nc.dram_tensor("dbg_" + name, list(shape), dt, kind="ExternalOutput").ap()
            dbg_out[name] = o
            dma_sp(o, ap, [ap], ["dbg_" + name], "dbg_" + name)

        def finish(extra=()):
            fw = [k for k in extra] + ["dbg_" + k for k in dbg_out]
            S.emit(final_wait=fw)
            return nc, dbg_out

        load_const(ident32, "ident")
        load_const(ident_bf, "ident", cast=True)
        load_const(ccol, "c_col")
        load_const(bcol, "b_col")
        load_const(g1c, "g1_col")
        load_const(g2c, "g2_col")
        load_const(qgc, "qg_col")
        load_const(kgc, "kg_col")
        load_const(c31, "c31")
        load_const(vmask, "vmask")
        load_const(kdec, "kdec")
        load_const(flag, "flag")
        memset(ones_bf, 1.0)
        memset(epsc, EPS)
        act(s_bf, ccol, AF.Silu)
        vcopy(s_bf2[:, :, 0], s_bf)
        vcopy(s_bf2[:, :, 1], s_bf)

        def build_s_rep(s_rep):
            for kc in range(NKC):
                ts(s_rep[:, kc, :], ones_bf, s_bf[:, kc:kc + 1], None, ALU.mult)

        if stop_after == "0":
            dump("s_bf", s_bf, [128, 16], BF16)
            return finish()
        def mod_col_section(sec_in_w, sec_out):
            ps = bank()
            for j in range(8):
                sl = slab_load(din["w_ada"], 0, NKC, sec_in_w * 2048 + j * 256)
                for m in range(2):
                    col = j * 2 + m
                    for kc in range(NKC):
                        mm(ps[:, 2 * col:2 * col + 2], sl[:, kc, m * 128:(m + 1) * 128], s_bf2[:, kc, :],
                           kc == 0, kc == NKC - 1)
            tt(modc[:, sec_out, :], ps[:, 0:32].rearrange("p (a b) -> p a b", b=2)[:, :, 0],
               bcol[:, sec_in_w * 16:(sec_in_w + 1) * 16], ALU.add)

        def mod_col_slab(sec_in_w, sec_out, j):
            sl = slab_load(din["w_ada"], 0, NKC, sec_in_w * 2048 + j * 256)
            ps = bank()
            for m in range(2):
                for kc in range(NKC):
                    mm(ps[:, 2 * m:2 * m + 2], sl[:, kc, m * 128:(m + 1) * 128], s_bf2[:, kc, :],
                       kc == 0, kc == NKC - 1)
            tt(modc[:, sec_out, 2 * j:2 * j + 2], ps[:, 0:4].rearrange("p (a b) -> p a b", b=2)[:, :, 0],
               bcol[:, sec_in_w * 16 + 2 * j:sec_in_w * 16 + 2 * j + 2], ALU.add)

        def mod_row_slab(sec_in_w, dst, s_rep, j):
            sl = slab_load(din["w_ada"], 0, NKC, sec_in_w * 2048 + j * 256)
            ps = bank()
            for kc in range(NKC):
                mm(ps[:, 0:256], s_rep[:, kc, :], sl[:, kc, :], kc == 0, kc == NKC - 1)
            evac_copy(dst[:, j * 256:(j + 1) * 256], ps[:, 0:256])

        def mod_row_section(sec_in_w, dst, s_rep):
            for j in range(8):
                sl = slab_load(din["w_ada"], 0, NKC, sec_in_w * 2048 + j * 256)
                ps = bank()
                for kc in range(NKC):
                    mm(ps[:, 0:256], s_rep[:, kc, :], sl[:, kc, :], kc == 0, kc == NKC - 1)
                evac_copy(dst[:, j * 256:(j + 1) * 256], ps[:, 0:256])

        import os as _os
        if _os.environ.get("SKIPA"):
            memset(modc[:, 0:2, :], 0.5)
        else:
            mod_col_section(1, 1)
            mod_col_section(0, 0)
        ts(A1c, modc[:, 1, :], 1.0, None, ALU.add)
        tt(A1c, A1c, g1c, ALU.mult)
        B1c = modc[:, 0, :]
        dump("modc", modc[:, 0:2, :], [128, 2, 16], F32)
        if stop_after == "A":
            return finish()

        hT = carve(hT_t, 0, BF16, [128, 2, NKC, 1024])

        def norm_tile(xt, junk, np_, si, Ac, Bc, dst_fn):
            act(junk[0:np_, :], xt, AF.Square, accum=ssq[0:np_, si:si + 1])
            act(rstd[0:np_, si:si + 1], ssq[0:np_, si:si + 1], AF.Sqrt, bias=epsc[0:np_, :], scale=1.0 / D)
            recip(rstd[0:np_, si:si + 1], rstd[0:np_, si:si + 1])
            ts(junk[0:np_, :], xt, rstd[0:np_, si:si + 1], None, ALU.mult)
            import os as _os
            if _os.environ.get("NB") == "1":
                dump("junk", junk, [128, 2048], BF16)
                return
            for q4 in range(int(_os.environ.get("NQ", "4"))):
                ps = bank()
                for j in range(4):
                    dc = q4 * 4 + j
                    tp(ps[:, j * 128:j * 128 + np_], junk[0:np_, dc * 128:(dc + 1) * 128], ident_bf[0:np_, 0:np_])
                for j in range(4):
                    dc = q4 * 4 + j
                    evac_affine(dst_fn(dc), ps[:, j * 128:j * 128 + np_], Ac[:, dc:dc + 1], Bc[:, dc:dc + 1])

        AA.off = 0
        xt_slots = [AA.alloc(F32, [128, 2048]) for _ in range(2)]
        junk = AA.alloc(BF16, [128, 2048])
        import os as _os
        for t in range(int(_os.environ.get("NT", "16"))):
            xt = xt_slots[t % 2]
            dma_sp(xt, din["xw"][t * 128:(t + 1) * 128, :], [], [xt], ("xt", t % 2))
            norm_tile(xt, junk, 128, t, A1c, B1c,
                      lambda dc, t=t: hT[:, t // 8, dc, (t % 8) * 128:(t % 8 + 1) * 128])
        dump("hT", hT_t[:, :], [128, 32768], BF16)
        dump("hTs", hT_t[:, 0:1024], [128, 1024], BF16)
        if stop_after == "B":
            return finish()

        def h_tok(qd):
            return lambda kc: hT[:, qd // 2, kc, (qd % 2) * 512:(qd % 2) * 512 + 512]

        def h_halo(kc):
            return hT[:, 0, kc, 1022:1024]

        def h_tile(t):
            return lambda kc: hT[:, t // 8, kc, (t % 8) * 128:(t % 8 + 1) * 128]

        def proj_fm(sl, m, rhs_fn, w=512, nk=NKC):
            ps = bank()
            for kc in range(nk):
                mm(ps[:, 0:w], sl[:, kc, m * 128:(m + 1) * 128], rhs_fn(kc), kc == 0, kc == nk - 1)
            return ps

        def proj_tm(sl, tiles, dst_fn, post=None, after_first=None):
            for i in range(0, len(tiles), 2):
                if i == 2 and after_first is not None:
                    after_first()
                ps = bank()
                for u in range(2):
                    lf = h_tile(tiles[i + u])
                    for kc in range(NKC):
                        mm(ps[:, u * 256:(u + 1) * 256], lf(kc), sl[:, kc, :], kc == 0, kc == NKC - 1)
                src = ps[:].rearrange("p (a b) -> p a b", a=2)
                if post is None:
                    evac_copy(dst_fn(i), src)
                else:
                    post(dst_fn(i), src)

        AA.off = 0
        qn = AA.alloc(BF16, [128, 4, 1024])
        kn = AA.alloc(BF16, [128, 4, 2048])
        va = AA.alloc(BF16, [128, 16, 512])
        R_END = AA.off
        yaT = AA.alloc(BF16, [128, 8, 1024])
        Y_END = AA.off
        BT = AA.alloc(F32, [128, 4, 3, 256])
        sq = [AA.alloc(BF16, [128, 512]) for _ in range(2)]
        f32a = [AA.alloc(F32, [128, 512]) for _ in range(2)]
        f32b = [AA.alloc(F32, [128, 512]) for _ in range(2)]
        pT = [AA.alloc(BF16, [128, 256]) for _ in range(3)]
        etmp = [AA.alloc(F32, [128, 256]) for _ in range(3)]
        rinv = AA.alloc(F32, [128, 256])
        e8 = AA.alloc(BF16, [8, 1024])
        nmT = AA.alloc(BF16, [8, 1024])
        qf32 = [AA.alloc(F32, [128, 512]) for _ in range(2)]
        load_const(e8, "e8", cast=True)
        tctr = [0]

        qk_pend = []

        def qknorm_flush():
            while qk_pend:
                i, qf, gcol, out, w = qk_pend.pop(0)
                ps2 = bank()
                mm(ps2[:, 0:w], ones_bf, sq[i][:, 0:w], True, True)
                ts(f32a[i][:, 0:w], ps2[:, 0:w], 1.0 / 128, EPS, ALU.mult, ALU.add)
                act(f32a[i][:, 0:w], f32a[i][:, 0:w], AF.Sqrt)
                recip(f32b[i][:, 0:w], f32a[i][:, 0:w])
                stt(out, qf, gcol, f32b[i][:, 0:w], ALU.mult, ALU.mult)

        def qknorm(ps, gcol, out, w=512):
            i = tctr[0] % 2
            tctr[0] += 1
            qf = qf32[i][:, 0:w]
            vcopy(qf, ps[:, 0:w])
            act(sq[i][:, 0:w], qf, AF.Square)
            qknorm_flush()
            qk_pend.append((i, qf, gcol, out, w))

        def gating(hl):
            v3 = kn[:, hl, :].rearrange("p (a b) -> p a b", a=8)
            S.op("dve", lambda e: e.tensor_reduce(km, v3, AX.X, ALU.add), reads=[kn[:, hl, :]], writes=[km])
            ts(km_bf, km, 1.0 / 256, None, ALU.mult)
            psg = psb[7]
            for qt in range(8):
                mm(psg[:, qt * 8:(qt + 1) * 8], qn[:, hl, qt * 128:(qt + 1) * 128], km_bf, True, True)
            tt(gm, psg[:, 0:64], vmask, ALU.add)
            for qt in range(8):
                S.op("dve", lambda e, qt=qt: e.max(mx8[:, qt * 8:(qt + 1) * 8], gm[:, qt * 8:(qt + 1) * 8]),
                     reads=[gm], writes=[mx8[:, qt * 8:(qt + 1) * 8]])
            for qt in range(8):
                ts(nm[:, qt * 8:(qt + 1) * 8], gm[:, qt * 8:(qt + 1) * 8],
                   mx8[:, qt * 8 + 2:qt * 8 + 3], NEG, ALU.is_lt, ALU.mult)
            tt(nm_bf, nm, vmask, ALU.add)
            pst = psb[7][:]
            for hq in range(2):
                for u in range(4):
                    qt = hq * 4 + u
                    tp(pst[0:8, u * 128:(u + 1) * 128], nm_bf[:, qt * 8:(qt + 1) * 8], ident_bf)
                vcopy(nmT[0:8, hq * 512:(hq + 1) * 512], pst[0:8, :])

        lctr = [0]

        def attn_block(hl, h, J, qs, w, btc0, mask_ap, psO, psS, out):
            kts = list(range(2 * (J + 1)))

            def qk(kt):
                n = kt // 2
                psL = psb[4 + lctr[0] % 3]
                pt = pT[lctr[0] % 3]
                lctr[0] += 1
                own = (n == J)
                use_mask = (not own) and (mask_ap is not None)
                mm(psL[:, 0:w], kn[:, hl, kt * 128:(kt + 1) * 128], qs, True, not use_mask)
                if use_mask:
                    mm(psL[:, 0:w], e8[0:8, n * 128:(n + 1) * 128], mask_ap, False, True)
                bti = None
                if own:
                    bti = kt - 2 * J
                elif n == J - 1 and kt % 2 == 1:
                    bti = 2
                et = etmp[lctr[0] % 3]
                if bti is None:
                    ts(et[:, 0:w], psL[:, 0:w], ATT_SCALE, c31[:, h:h + 1], ALU.mult, ALU.add)
                else:
                    stt(et[:, 0:w], psL[:, 0:w], ATT_SCALE, BT[:, hl, bti, btc0:btc0 + w], ALU.mult, ALU.add)
                act(pt[:, 0:w], et[:, 0:w], AF.Exp)
                return pt

            def pv(kt, pt, first, last):
                mm(psO[:, 0:w], va[:, kt, hl * 128:(hl + 1) * 128], pt[:, 0:w], first, last)
                mm(psS[:, 0:w], ones_bf, pt[:, 0:w], first, last)

            pend = []
            for kt in kts:
                pend.append((kt, qk(kt)))
                if len(pend) > 2:
                    k0, p0 = pend.pop(0)
                    pv(k0, p0, k0 == kts[0], False)
            while pend:
                k0, p0 = pend.pop(0)
                pv(k0, p0, k0 == kts[0], len(pend) == 0)
            recip(rinv[:, 0:w], psS[:, 0:w])
            tt(out, psO[:, 0:w], rinv[:, 0:w], ALU.mult)

        def attention(hl, h):
            for jq in range(4):
                attn_block(hl, h, 4 + jq, qn[:, hl, jq * 256:(jq + 1) * 256], 256, 0,
                           nmT[0:8, jq * 256:(jq + 1) * 256], psb[jq % 2], psb[2 + jq % 2],
                           yaT[:, h, jq * 256:(jq + 1) * 256])
            attn_block(hl, h, 3, qnh[:, hl, :], 2, 254, None, psb[0], psb[2], yaTh[:, h, :])

        for g in range(2):
            dma_sp(BT, din["bt"][:, 4 * g:4 * g + 4], [], [BT], "bt")
            for s in range(2):
                sl = slab_load(din["w_in"], 0, NKC, g * 512 + s * 256)
                for m in range(2):
                    for th in range(2):
                        ps = proj_fm(sl, m, h_tok(2 + th))
                        qknorm(ps, qgc[:, 0:1], qn[:, 2 * s + m, th * 512:(th + 1) * 512])
                    ps = proj_fm(sl, m, h_halo, w=2)
                    qknorm(ps, qgc[:, 0:1], qnh[:, 2 * s + m, :], w=2)
            for s in range(2):
                sl = slab_load(din["w_in"], 0, NKC, 1024 + g * 512 + s * 256)
                for m in range(2):
                    for qd in range(4):
                        ps = proj_fm(sl, m, h_tok(qd))
                        qknorm(ps, kgc[:, 0:1], kn[:, 2 * s + m, qd * 512:(qd + 1) * 512])
            for s in range(2):
                sl = slab_load(din["w_in"], 0, NKC, 2048 + g * 512 + s * 256)
                proj_tm(sl, list(range(16)), lambda i, s=s: va[:, i:i + 2, s * 256:(s + 1) * 256],
                        after_first=qknorm_flush)
            qknorm_flush()
            if g == 0:
                dump("qn", qn, [128, 4, 1024], BF16)
                dump("kn", kn, [128, 4, 2048], BF16)
                dump("va", va, [128, 16, 512], BF16)
            gating(0)
            if g == 0:
                dump("nmT", nmT, [8, 1024], BF16)
            for hl in range(4):
                attention(hl, 4 * g + hl)
                if hl < 3:
                    gating(hl + 1)
        dump("yaT", yaT, [128, 8, 1024], BF16)
        dump("yaTh", yaTh, [128, 8, 2], BF16)
        if stop_after == "C":
            return finish()

        AA.off = 0
        qrT = AA.alloc(BF16, [128, 2, 1024])
        krT = AA.alloc(BF16, [128, 2, 2048])
        kdT = AA.alloc(BF16, [128, 2, 16, 128])
        vr = AA.alloc(BF16, [128, 16, 256])
        Sb = AA.alloc(BF16, [128, 8, 256])
        sT = AA.alloc(BF16, [128, 8, 128])
        qdT = AA.alloc(BF16, [128, 8, 128])
        assert AA.off <= R_END, AA.off
        AA.off = Y_END
        zt = AA.alloc(BF16, [128, 8, 512])
        sg = AA.alloc(BF16, [128, 8, 256])
        zst = AA.alloc(BF16, [128, 2, 1024])
        sgh = AA.alloc(BF16, [128, 256])
        Sbh = AA.alloc(BF16, [128, 256])
        zth = AA.alloc(BF16, [128, 512])
        ynh = AA.alloc(F32, [128, 256])
        cs = AA.alloc(F32, [128, 2, 512])
        rgb = AA.alloc(F32, [128, 512])
        decTh = AA.alloc(F32, [128, 128])
        qdech = AA.alloc(F32, [128, 128])
        Sst = AA.alloc(F32, [128, 256])
        r32a = [AA.alloc(F32, [128, 512]) for _ in range(2)]
        r32b = [AA.alloc(F32, [128, 512]) for _ in range(2)]
        yn = [AA.alloc(F32, [128, 256]) for _ in range(2)]
        sgtmp = AA.alloc(F32, [128, 2, 256])
        rctr = [0]

        def rotary(ps, out, c0=0, w=512):
            i = rctr[0] % 2
            rctr[0] += 1
            a, b2 = r32a[i], r32b[i]
            tt(a[:, 0:w], ps[:, 0:w], cs[:, 0, c0:c0 + w], ALU.mult)
            tt(b2[0:64, 0:w], ps[64:128, 0:w], cs[64:128, 1, c0:c0 + w], ALU.mult)
            tt(b2[64:128, 0:w], ps[0:64, 0:w], cs[0:64, 1, c0:c0 + w], ALU.mult)
            tt(out, a[:, 0:w], b2[:, 0:w], ALU.add)

        def groupnorm_gate(o, np_, bi, gslice, sgv, ytmp, dst):
            S.op("dve", lambda e: e.bn_stats(bnst[0:np_, bi, :], o), reads=[o], writes=[bnst[0:np_, bi, :]])
            S.op("dve", lambda e: e.bn_aggr(bnmv[0:np_, bi, :], bnst[0:np_, bi, :]),
                 reads=[bnst[0:np_, bi, :]], writes=[bnmv[0:np_, bi, :]])
            act(smallf[0:np_, bi:bi + 1], bnmv[0:np_, bi, 1:2], AF.Sqrt, bias=epsc[0:np_, :], scale=1.0)
            recip(smallf[0:np_, bi:bi + 1], smallf[0:np_, bi:bi + 1])
            ts(ytmp, o, bnmv[0:np_, bi, 0:1], smallf[0:np_, bi:bi + 1], ALU.subtract, ALU.mult)
            tt(ytmp, ytmp, gslice, ALU.mult)
            tt(dst, ytmp, sgv, ALU.mult)

        for rg in range(4):
            slq = slab_load(din["w_in"], 0, NKC, 3072 + rg * 256)
            slk = slab_load(din["w_in"], 0, NKC, 4096 + rg * 256)
            dma_sp(rgb, din["rg_bc"][:, rg * 512:(rg + 1) * 512], [], [rgb], "rgb")
            for qd in range(4):
                dma_sp(cs[:, 0, :], din["cosT"][:, qd * 512:(qd + 1) * 512], [], [cs[:, 0, :]], "cs0")
                dma_sp(cs[:, 1, :], din["sinT"][:, qd * 512:(qd + 1) * 512], [], [cs[:, 1, :]], "cs1")
                for m in range(2):
                    ps = proj_fm(slk, m, h_tok(qd))
                    rotary(ps, krT[:, m, qd * 512:(qd + 1) * 512])
                if qd == 1:
                    for m in range(2):
                        ps = proj_fm(slq, m, h_halo, w=2)
                        rotary(ps, qrTh[:, m, :], c0=510, w=2)
                if qd >= 2:
                    for m in range(2):
                        ps = proj_fm(slq, m, h_tok(qd))
                        rotary(ps, qrT[:, m, (qd - 2) * 512:(qd - 1) * 512])
            if rg == 0:
                dump("qrT", qrT, [128, 2, 1024], BF16)
                dump("krT", krT, [128, 2, 2048], BF16)
            for m in range(2):
                h = 2 * rg + m
                for qq in range(4):
                    pst = bank()
                    for u in range(4):
                        t = qq * 4 + u
                        tp(pst[:, u * 128:(u + 1) * 128], krT[:, m, t * 128:(t + 1) * 128], ident_bf)
                    dst = kdT[:, m, qq * 4:(qq + 1) * 4, :]
                    src = pst.rearrange("p (a b) -> p a b", a=4)
                    ts(dst, src, kdec[:, h:h + 1], None, ALU.mult)
            for m in range(2):
                h = 2 * rg + m
                dma_sp(decTh, din["decT"][:, h, :], [], [decTh], "decTh")
                dma_sp(qdech, din["qdec"][:, h, :], [], [qdech], "qdech")
                slv = slab_load(din["w_in"], 0, NKC, 5120 + h * 256)
                proj_tm(slv, list(range(16)), lambda i: vr[:, i:i + 2, :])
                slg = slab_load(din["w_in"], 0, NKC, 7168 + h * 256)
                def _silu_post(d, s_):
                    vcopy(sgtmp, s_)
                    act(d, sgtmp, AF.Silu)
                proj_tm(slg, list(range(8, 16)), lambda i: sg[:, i:i + 2, :], post=_silu_post)
                psh = bank()
                for kc in range(NKC):
                    mm(psh[0:2, 0:256], h_halo(kc), slg[:, kc, :], kc == 0, kc == NKC - 1)
                vcopy(ynh[0:2, :], psh[0:2, 0:256])
                act(sgh[0:2, :], ynh[0:2, :], AF.Silu)
                for n in range(15):
                    o = bank()[:, 0:256]
                    mm(o, kdT[:, m, n, :], vr[:, n, :], True, True)
                    if n == 0:
                        vcopy(Sst, o)
                    else:
                        if n == 7:
                            acopy(Sbh, Sst)
                        if n == 8:
                            ts(Sst, Sst, flag[:, 0:1], None, ALU.mult)
                        if n >= 8:
                            acopy(Sb[:, n - 8, :], Sst)
                        stt(Sst, Sst, cd[h], o, ALU.mult, ALU.add)
                acopy(Sb[:, 7, :], Sst)
                for c4 in range(2):
                    pss = bank()
                    for u in range(4):
                        c = c4 * 4 + u
                        mm(pss[:, u * 128:(u + 1) * 128], krT[:, m, (8 + c) * 128:(9 + c) * 128],
                           qrT[:, m, c * 128:(c + 1) * 128], True, True)
                    for u in range(4):
                        c = c4 * 4 + u
                        tt(sT[:, c, :], pss[:, u * 128:(u + 1) * 128], decTh, ALU.mult)
                for c in range(8):
                    tt(qdT[:, c, :], qrT[:, m, c * 128:(c + 1) * 128], qdech, ALU.mult)
                for c in range(8):
                    o = bank()[:, 0:256]
                    mm(o, sT[:, c, :], vr[:, 8 + c, :], True, False)
                    mm(o, qdT[:, c, :], Sb[:, c, :], False, True)
                    groupnorm_gate(o, 128, c % 2, rgb[:, m * 256:(m + 1) * 256], sg[:, c, :], yn[c % 2],
                                   zt[:, c, m * 256:(m + 1) * 256])
                pss = bank()
                mm(pss[:, 0:2], krT[:, m, 7 * 128:8 * 128], qrTh[:, m, :], True, True)
                tt(sTh, pss[:, 0:2], decTh[:, 126:128], ALU.mult)
                tt(qdTh, qrTh[:, m, :], qdech[:, 126:128], ALU.mult)
                psy = bank()
                mm(psy[0:2, 0:256], sTh, vr[:, 7, :], True, False)
                mm(psy[0:2, 0:256], qdTh, Sbh, False, True)
                groupnorm_gate(psy[0:2, 0:256], 2, 2, rgb[0:2, m * 256:(m + 1) * 256], sgh[0:2, :],
                               ynh[0:2, :], zth[0:2, m * 256:(m + 1) * 256])
            if rg == 0:
                dump("zt", zt, [128, 8, 512], BF16)
            for fc in range(4):
                for hc in range(2):
                    pst = bank()
                    for u in range(4):
                        c = hc * 4 + u
                        tp(pst[:, u * 128:(u + 1) * 128], zt[:, c, fc * 128:(fc + 1) * 128], ident_bf)
                    evac_copy(zst[:, fc % 2, hc * 512:(hc + 1) * 512], pst)
                if fc % 2 == 1:
                    a0 = rg * 4 + fc - 1
                    dma_sp(zTs[a0:a0 + 2].rearrange("a p t -> p a t"), zst, [zst], ["zTs"], "zst")
            psth = bank()
            for fc in range(4):
                tp(psth[:, fc * 2:fc * 2 + 2], zth[0:2, fc * 128:(fc + 1) * 128], ident_bf[0:2, 0:2])
            vcopy(zTh[:, rg * 4:(rg + 1) * 4, :], psth[:, 0:8].rearrange("p (a b) -> p a b", a=4))
        dump("zTh", zTh, [128, 16, 2], BF16)
        if stop_after == "D":
            return finish()

        zT = carve(hT_t, 0, BF16, [128, 16, 1024])
        hTh = CA.alloc(BF16, [128, 16, 2])
        vcopy(hTh, hT[:, 0, :, 1022:1024])
        dma_sp(zT, zTs.rearrange("a p t -> p a t"), ["zTs"], [zT], "zTl")
        AA.off = Y_END
        mT = AA.alloc(BF16, [128, 16, 1024])
        AA.off = 0
        g1bc = AA.alloc(F32, [128, 2048])
        s_rep = AA.alloc(BF16, [128, 16, 128])
        F_BASE = AA.off
        build_s_rep(s_rep)
        gA = [AA.alloc(F32, [128, 512]) for _ in range(4)]
        gB = [AA.alloc(F32, [128, 512]) for _ in range(4)]
        gAh = AA.alloc(F32, [128, 2, 2])
        gBh = AA.alloc(F32, [128, 2, 2])
        assert AA.off <= R_END
        grp = [(m, th) for m in range(2) for th in range(2)]

        def gate_pass(sl, dstl, dsth):
            for gi, (m, th) in enumerate(grp):
                p = proj_fm(sl, m, h_tok(2 + th))
                vcopy(dstl[gi], p)
                act(dstl[gi], dstl[gi], AF.Sigmoid)
            for m in range(2):
                p = proj_fm(sl, m, lambda kc: hTh[:, kc, :], w=2)
                vcopy(dsth[:, m, :], p[:, 0:2])
                act(dsth[:, m, :], dsth[:, m, :], AF.Sigmoid)

        for dg in range(8):
            slga = slab_load(din["w_in"], 0, NKC, 9216 + dg * 256)
            gate_pass(slga, gA, gAh)
            slgb = slab_load(din["w_in"], 0, NKC, 11264 + dg * 256)
            gate_pass(slgb, gB, gBh)
            sla = slab_load(din["w_attn_br"], 0, 8, dg * 256)
            for gi, (m, th) in enumerate(grp):
                tsl = slice(th * 512, (th + 1) * 512)
                p = proj_fm(sla, m, lambda kc, tsl=tsl: yaT[:, kc, tsl], nk=8)
                tt(gA[gi], p, gA[gi], ALU.mult)
            for m in range(2):
                p = proj_fm(sla, m, lambda kc: yaTh[:, kc, :], w=2, nk=8)
                tt(gAh[:, m, :], p[:, 0:2], gAh[:, m, :], ALU.mult)
            mod_row_slab(2, g1bc, s_rep, dg)
            slr = slab_load(din["w_ret_br"], 0, NKC, dg * 256)
            for gi, (m, th) in enumerate(grp):
                tsl = slice(th * 512, (th + 1) * 512)
                p = proj_fm(slr, m, lambda kc, tsl=tsl: zT[:, kc, tsl])
                tt(gB[gi], p, gB[gi], ALU.mult)
                tt(mT[:, dg * 2 + m, tsl], gA[gi], gB[gi], ALU.add)
            for m in range(2):
                p = proj_fm(slr, m, lambda kc: zTh[:, kc, :], w=2)
                tt(gBh[:, m, :], p[:, 0:2], gBh[:, m, :], ALU.mult)
                tt(mTh[:, dg * 2 + m, :], gAh[:, m, :], gBh[:, m, :], ALU.add)
        dump("mT", mT, [128, 16, 1024], BF16)
        dump("mTh", mTh, [128, 16, 2], BF16)
        if stop_after == "E":
            return finish()

        AA.off = F_BASE
        btmp = AA.alloc(F32, [128, 2048])
        xin = [AA.alloc(F32, [128, 8, 256]) for _ in range(2)]
        xh = AA.alloc(F32, [128, 2048])
        assert AA.off <= Y_END
        dma_sp(btmp, din["b_g1"], [], [btmp], "btmp")
        dma_sp(xh[0:2, :], din["xw"][1022:1024, :], [], [xh[0:2, :]], "xh")
        assert AA.off <= Y_END
        tt(g1bc, g1bc, btmp, ALU.add)
        xown = din["xw"][1024:2048, :]
        for dg in range(8):
            dsl = slice(dg * 256, (dg + 1) * 256)
            sl = slab_load(din["w_o"], 0, NKC, dg * 256)
            xi = xin[dg % 2]
            dma_sp(xi, xown[:, dsl].rearrange("(t p) n -> p t n", p=128), [], [xi], ("xin", dg % 2))
            for t2 in range(4):
                ps = bank()
                for u in range(2):
                    t = t2 * 2 + u
                    for kc in range(NKC):
                        mm(ps[:, u * 256:(u + 1) * 256], mT[:, kc, t * 128:(t + 1) * 128], sl[:, kc, :],
                           kc == 0, kc == NKC - 1)
                for u in range(2):
                    tt(ps[:, u * 256:(u + 1) * 256], ps[:, u * 256:(u + 1) * 256], g1bc[:, dsl], ALU.mult)
                src = ps[:].rearrange("p (a b) -> p a b", a=2)
                tt(xi[:, t2 * 2:t2 * 2 + 2, :], xi[:, t2 * 2:t2 * 2 + 2, :], src, ALU.add)
            ps = bank()
            for kc in range(NKC):
                mm(ps[0:2, 0:256], mTh[:, kc, :], sl[:, kc, :], kc == 0, kc == NKC - 1)
            tt(ps[0:2, 0:256], ps[0:2, 0:256], g1bc[0:2, dsl], ALU.mult)
            tt(xh[0:2, dsl], xh[0:2, dsl], ps[0:2, 0:256], ALU.add)
            dma_sp(x1s[:, dsl].rearrange("(t p) n -> p t n", p=128), xi, [xi], [("x1s", dg)], ("x1w", dg % 2))
            mod_col_slab(4, 3, dg)
            mod_col_slab(3, 2, dg)
        if "x1" in debug:
            xd = AA.alloc(F32, [128, 2048])
            dma_sp(xd, x1s[0:128, :], X1KEYS, [xd], "xd")
            dump("x1", xd, [128, 2048], F32)
            dump("xh", xh[0:2, :], [2, 2048], F32)
        if stop_after == "F":
            return finish()

        ts(A2c, modc[:, 3, :], 1.0, None, ALU.add)
        tt(A2c, A2c, g2c, ALU.mult)
        B2c = modc[:, 2, :]
        h2T = carve(hT_t, 32768, BF16, [128, NKC, 1024])
        HA.off = 0
        xt2 = [HA.alloc(F32, [128, 2048]) for _ in range(2)]
        junk2 = HA.alloc(BF16, [128, 2048])
        assert HA.off <= 32768
        for t in range(8):
            xt = xt2[t % 2]
            dma_sp(xt, x1s[t * 128:(t + 1) * 128, :], X1KEYS, [xt], ("xt2", t % 2))
            norm_tile(xt, junk2, 128, t, A2c, B2c, lambda dc, t=t: h2T[:, dc, t * 128:(t + 1) * 128])
        norm_tile(xh[0:2, :], junk2, 2, 16, A2c, B2c, lambda dc: h2Th[:, dc, :])
        dump("h2T", h2T, [128, NKC, 1024], BF16)
        dump("h2Th", h2Th, [128, NKC, 2], BF16)
        if stop_after == "G":
            return finish()

        actT = carve(A_t, 0, BF16, [128, NFC, 1024])
        HA.off = 0
        cwt = HA.alloc(F32, [128, 88, 3])
        cbt = HA.alloc(F32, [128, 88])
        uv = [HA.alloc(F32, [128, 1024]) for _ in range(2)]
        ug = [HA.alloc(F32, [128, 1024]) for _ in range(2)]
        sgt = [HA.alloc(F32, [128, 1024]) for _ in range(2)]
        s_rep2 = HA.alloc(BF16, [128, 16, 128])
        assert HA.off <= 32768, HA.off
        g2bc = carve(A_t, 90112, F32, [128, 2048])
        build_s_rep(s_rep2)
        dma_sp(cwt, din["cw"], [], [cwt], "cwt")
        dma_sp(cbt, din["cb"], [], [cbt], "cbt")

        def conv(dst, psl, ph, ch):
            w0, w1, w2 = cwt[:, ch, 0:1], cwt[:, ch, 1:2], cwt[:, ch, 2:3]
            for th in range(2):
                o = dst[:, th * 512:(th + 1) * 512]
                ts(o, psl[th], w2, cbt[:, ch:ch + 1], ALU.mult, ALU.add)
            for th in range(2):
                b0 = th * 512
                stt(dst[:, b0 + 1:b0 + 512], psl[th][:, 0:511], w1, dst[:, b0 + 1:b0 + 512], ALU.mult, ALU.add)
                stt(dst[:, b0 + 2:b0 + 512], psl[th][:, 0:510], w0, dst[:, b0 + 2:b0 + 512], ALU.mult, ALU.add)
            stt(dst[:, 512:513], psl[0][:, 511:512], w1, dst[:, 512:513], ALU.mult, ALU.add)
            stt(dst[:, 512:514], psl[0][:, 510:512], w0, dst[:, 512:514], ALU.mult, ALU.add)
            stt(dst[:, 0:1], ph[:, 1:2], w1, dst[:, 0:1], ALU.mult, ALU.add)
            stt(dst[:, 0:2], ph[:, 0:2], w0, dst[:, 0:2], ALU.mult, ALU.add)

        def up_chunk(sl, m, ch, dst, hi):
            ps2 = [proj_fm(sl, m, lambda kc, th=th: h2T[:, kc, th * 512:(th + 1) * 512]) for th in range(2)]
            psh = proj_fm(sl, m, lambda kc: h2Th[:, kc, :], w=2)
            ts(Phs[:, hi, :], psh[:, 0:2], flag[:, 0:1], None, ALU.mult)
            conv(dst, ps2, Phs[:, hi, :], ch)

        fctr = [0]
        for fp in range(22):
            g2sched = {1: 0, 4: 1, 7: 2, 10: 3, 13: 4, 16: 5, 19: 6, 21: 7}
            if fp in g2sched:
                mod_row_slab(5, g2bc, s_rep2, g2sched[fp])
            slv = slab_load(din["w_up"], 0, NKC, fp * 256)
            slg = slab_load(din["w_up"], 0, NKC, FFN + fp * 256)
            for m in range(2):
                fc = fp * 2 + m
                i = fctr[0] % 2
                fctr[0] += 1
                up_chunk(slv, m, fc, uv[i], 0)
                up_chunk(slg, m, 44 + fc, ug[i], 1)
                act(sgt[i], ug[i], AF.Silu)
                tt(actT[:, fc, :], uv[i], sgt[i], ALU.mult)
        dump("actT", actT, [128, NFC, 1024], BF16)
        if stop_after == "H":
            return finish()

        HA.off = 0
        btmp2 = HA.alloc(F32, [128, 2048])
        xo = [HA.alloc(F32, [128, 8, 256]) for _ in range(2)]
        assert HA.off <= 32768
        dma_sp(btmp2, din["b_g2"], [], [btmp2], "btmp2")
        tt(g2bc, g2bc, btmp2, ALU.add)
        ykeys = []
        for dg in range(8):
            dsl = slice(dg * 256, (dg + 1) * 256)
            xi = xo[dg % 2]
            dma_sp(xi, x1s[:, dsl].rearrange("(t p) n -> p t n", p=128), X1KEYS, [xi], ("xo", dg % 2))
            for (k0, nk) in [(0, 16), (16, 16), (32, 12)]:
                sl = slab_load(din["w_down"], k0 * 128, nk, dg * 256)
                for t in range(8):
                    o = psb[t][:, 0:256]
                    for kk in range(nk):
                        kc = k0 + kk
                        mm(o, actT[:, kc, t * 128:(t + 1) * 128], sl[:, kk, :], kc == 0, kc == NFC - 1)
            for t in range(8):
                ps = psb[t]
                tt(ps[:, 0:256], ps[:, 0:256], g2bc[:, dsl], ALU.mult)
                tt(xi[:, t, :], xi[:, t, :], ps[:, 0:256], ALU.add)
            dma_sp(y[:, dsl].rearrange("(t p) n -> p t n", p=128), xi, [xi], [("y", dg)], ("yw", dg))
            ykeys.append(("yw", dg))
        return finish(extra=ykeys)


_CACHE = {}


def input_names(nc):
    out = []
    for a in nc.m.functions[0].allocations:
        if isinstance(a, mybir.MemoryLocationSet) and a.kind == "ExternalInput":
            out.append(a.memorylocations[0].name)
    return out


def kernel(**inputs):
    inputs = {k: np.asarray(v) for k, v in inputs.items()}
    if "nc" not in _CACHE:
        _CACHE["nc"] = build_program()[0]
        _CACHE["names"] = input_names(_CACHE["nc"])
    nc = _CACHE["nc"]
    names = set(_CACHE["names"])
    in_maps = [{k: v for k, v in make_core_inputs(inputs, c).items() if k in names} for c in range(8)]
    res = run_bass_kernel_spmd(nc, in_maps, core_ids=list(range(8)))
    out = np.empty((4, 2048, D), np.float32)
    for c in range(8):
        out[c // 2, (c % 2) * 1024:(c % 2 + 1) * 1024, :] = res.results[c]["y"]
    return out
```

```python
import contextlib
import math

import numpy as np
import concourse.bass as bass
import concourse.mybir as mybir
from concourse.bass_utils import run_bass_kernel_spmd

F32 = mybir.dt.float32
BF16 = mybir.dt.bfloat16
ALU = mybir.AluOpType
AF = mybir.ActivationFunctionType
AX = mybir.AxisListType

D = 2048
OWN = 1024
WIN = 2048
NKC = 16
NEG = -30000.0
EPS = 1e-6
FFN = 5632
NFC = 44
ATT_SCALE = 128 ** -0.5
ENGS = ("pe", "act", "dve", "pool", "sp")
GRAN = 512


def _esz(dt):
    return 4 if dt == F32 else 2


class Sched:
    def __init__(self, nc):
        self.nc = nc
        self.ops = {e: [] for e in ENGS}
        self.last_w = {}
        self.readers = {}
        self.dma_cnt = {}

    @staticmethod
    def keys(x):
        if isinstance(x, (str, tuple)):
            return [x]
        ap = x.ap
        esz = _esz(x.dtype)
        pstride = ap[0][0]
        off = x.offset % pstride if pstride > 0 else x.offset
        ext = 1
        for (st, cnt) in ap[1:]:
            ext += abs(st) * (cnt - 1)
        lo = off * esz
        hi = (off + ext) * esz
        name = x.tensor.name
        if name.startswith("ps"):
            return [(name, 0)]
        return [(name, g) for g in range(lo // GRAN, (hi - 1) // GRAN + 1)]

    def op(self, eng, fn, reads=(), writes=(), dma=None):
        idx = len(self.ops[eng])
        me = (eng, idx)
        deps = set()
        rk = [k for r in reads for k in self.keys(r)]
        wk = [k for w in writes for k in self.keys(w)]
        for k in rk:
            w = self.last_w.get(k)
            if w is not None:
                deps.add(w)
        for k in wk:
            w = self.last_w.get(k)
            if w is not None:
                deps.add(w)
            for r in self.readers.get(k, {}).items():
                deps.add(r)
        deps.discard(me)
        rec = dict(fn=fn, deps=deps, dma=dma, signal=False, cnt=None)
        if dma is not None:
            self.dma_cnt[dma] = self.dma_cnt.get(dma, 0) + 1
            rec["cnt"] = 16 * self.dma_cnt[dma]
            rec["signal"] = True
        self.ops[eng].append(rec)
        for k in rk:
            self.readers.setdefault(k, {})[eng] = idx
        for k in wk:
            self.last_w[k] = me
            self.readers[k] = {}
        return me

    def emit(self, final_wait=()):
        nc = self.nc
        ops = self.ops
        for e in ENGS:
            for rec in ops[e]:
                nd = set()
                for (de, di) in rec["deps"]:
                    drec = ops[de][di]
                    if de == e and drec["dma"] is None and e in ("pe", "sp"):
                        continue
                    nd.add((de, di))
                rec["deps"] = nd
                for (de, di) in nd:
                    if ops[de][di]["dma"] is None:
                        ops[de][di]["signal"] = True
        for e in ENGS:
            c = 0
            for rec in ops[e]:
                if rec["dma"] is None and rec["signal"]:
                    c += 1
                    rec["cnt"] = c
        with contextlib.ExitStack() as st:
            esem = {e: st.enter_context(nc.semaphore("s_" + e)) for e in ENGS}
            dsem = {k: st.enter_context(nc.semaphore("d_%d" % i))
                    for i, k in enumerate(self.dma_cnt)}
            block = st.enter_context(nc.Block())

            def run(e):
                def body(engobj):
                    waited = {}
                    for rec in ops[e]:
                        need = {}
                        for (de, di) in rec["deps"]:
                            drec = ops[de][di]
                            s = ("d", drec["dma"]) if drec["dma"] is not None else ("e", de)
                            need[s] = max(need.get(s, 0), drec["cnt"])
                        for s, v in need.items():
                            if waited.get(s, 0) >= v:
                                continue
                            waited[s] = v
                            sem = dsem[s[1]] if s[0] == "d" else esem[s[1]]
                            engobj.wait_ge(sem, v)
                        ins = rec["fn"](engobj)
                        if rec["dma"] is not None:
                            ins.then_inc(dsem[rec["dma"]], 16)
                        elif rec["signal"]:
                            ins.then_inc(esem[e], 1)
                    if e == "sp":
                        for k in final_wait:
                            engobj.wait_ge(dsem[k], 16 * self.dma_cnt[k])
                return body

            block.tensor(run("pe"))
            block.scalar(run("act"))
            block.vector(run("dve"))
            block.gpsimd(run("pool"))
            block.sync(run("sp"))


def _t5_bucket(dist):
    n = np.maximum(dist, 0)
    max_exact = 16
    nf = np.maximum(n, 1).astype(np.float32)
    large = max_exact + (np.log(nf / max_exact) / math.log(128 / max_exact) * (32 - max_exact)).astype(np.int32)
    large = np.minimum(large, 31)
    return np.where(n < max_exact, n, large)


def _const_tables(half):
    out = {}
    k = np.arange(128)[:, None]
    q = np.arange(256)[None, :]
    dists = [q - k, q - k - 128, q - k + 128]
    out["bt_bucket"] = [_t5_bucket(d) for d in dists]
    out["bt_valid"] = [d >= 0 for d in dists]
    vm = np.full((8, 8), NEG, np.float32)
    for qt in range(8):
        j = qt // 2
        for n in range(8):
            ok = (n < 4 + j) and (n >= 4 or half == 1)
            if ok:
                vm[qt, n] = 0.0
    out["vmask"] = np.ascontiguousarray(np.broadcast_to(vm.reshape(1, 64), (128, 64))).astype(np.float32)
    e8 = np.zeros((8, 8, 128), np.float32)
    for n in range(8):
        e8[n, n, :] = 1.0
    out["e8"] = e8.reshape(8, 1024)
    pos = (np.arange(WIN) - 1024 + half * 1024).astype(np.float32)
    freqs = np.power(np.float32(10000.0), -np.arange(64, dtype=np.float32) / 64).astype(np.float32)
    ang = (pos[None, :] * freqs[:, None]).astype(np.float32)
    cos = np.cos(ang).astype(np.float32)
    sin = np.sin(ang).astype(np.float32)
    out["cosT"] = np.concatenate([cos, cos], 0)
    out["sinT"] = np.concatenate([sin, -sin], 0)
    hh = np.arange(8, dtype=np.float32)
    log_decay = np.log(1.0 - np.power(2.0, -5.0 - hh)).astype(np.float32)
    i = np.arange(128, dtype=np.float32)
    diff = i[:, None] - i[None, :]
    inner = np.where(diff >= 0, np.exp(log_decay[:, None, None] * np.maximum(diff, 0.0)), 0.0)
    sc = np.float32(128 ** -0.5)
    out["decT"] = np.ascontiguousarray(inner.transpose(2, 0, 1) * sc).astype(np.float32)
    qd = np.exp(log_decay[:, None] * (i + 1.0)).astype(np.float32)
    out["qdec"] = np.ascontiguousarray(np.broadcast_to(qd[None], (128, 8, 128))).astype(np.float32)
    kd = np.exp(log_decay[:, None] * (127.0 - i)).astype(np.float32) * sc
    out["kdec"] = np.ascontiguousarray(kd.T).astype(np.float32)
    out["chunk_decay"] = [float(np.exp(np.float32(log_decay[h] * 128.0))) for h in range(8)]
    out["flag"] = np.full((128, 1), float(half), np.float32)
    return out


def _col(v, n):
    return np.ascontiguousarray(np.asarray(v, np.float32).reshape(n, 128).T)


def _bc(v):
    v = np.asarray(v, np.float32).reshape(1, -1)
    return np.ascontiguousarray(np.broadcast_to(v, (128, v.shape[1])))


def make_core_inputs(inp, core):
    b, half = core // 2, core % 2
    ct = _const_tables(half)
    x = np.asarray(inp["x"], np.float32)
    m = {}
    if half == 1:
        m["xw"] = np.ascontiguousarray(x[b])
    else:
        m["xw"] = np.ascontiguousarray(np.concatenate([np.zeros((1024, D), np.float32), x[b, :1024]], 0))
    m["c_col"] = _col(inp["c"][b], 16)
    m["w_ada"] = np.ascontiguousarray(inp["w_ada"][0])
    bada = np.asarray(inp["b_ada"][0], np.float32)
    m["b_col"] = _col(bada, 96)
    m["b_g1"] = _bc(bada[4096:6144])
    m["b_g2"] = _bc(bada[10240:12288])
    m["g1_col"] = _col(inp["norm1_g"][0], 16)
    m["g2_col"] = _col(inp["norm2_g"][0], 16)
    m["w_in"] = np.ascontiguousarray(inp["w_in"][0])
    m["qg_col"] = _col(inp["q_norm_g"][0], 1)
    m["kg_col"] = _col(inp["k_norm_g"][0], 1)
    rb = np.asarray(inp["rel_bias"], np.float32)
    bt = np.empty((128, 8, 3, 256), np.float32)
    for j in range(3):
        g = rb[ct["bt_bucket"][j]]
        g = np.where(ct["bt_valid"][j][:, :, None], g, np.float32(NEG))
        bt[:, :, j, :] = g.transpose(0, 2, 1)
    m["bt"] = bt
    m["c31"] = _bc(rb[31])
    m["vmask"] = ct["vmask"]
    m["e8"] = ct["e8"]
    m["rg_bc"] = _bc(inp["ret_norm_g"][0])
    m["cosT"] = ct["cosT"]
    m["sinT"] = ct["sinT"]
    m["decT"] = ct["decT"]
    m["qdec"] = ct["qdec"]
    m["kdec"] = ct["kdec"]
    m["flag"] = ct["flag"]
    m["w_attn_br"] = np.ascontiguousarray(inp["w_attn_br"][0])
    m["w_ret_br"] = np.ascontiguousarray(inp["w_ret_br"][0])
    m["w_o"] = np.ascontiguousarray(inp["w_o"][0])
    m["w_up"] = np.ascontiguousarray(inp["w_up"][0])
    cw = np.asarray(inp["conv_w"][0], np.float32)
    m["cw"] = np.ascontiguousarray(cw.reshape(3, 88, 128).transpose(2, 1, 0))
    m["cb"] = _col(inp["conv_b"][0], 88)
    m["w_down"] = np.ascontiguousarray(inp["w_down"][0])
    m["ident"] = np.eye(128, dtype=np.float32)
    return m


INPUT_SHAPES = {
    "xw": [2048, 2048], "c_col": [128, 16], "w_ada": [2048, 12288], "b_col": [128, 96],
    "b_g1": [128, 2048], "b_g2": [128, 2048], "g1_col": [128, 16], "g2_col": [128, 16],
    "w_in": [2048, 13312], "qg_col": [128, 1], "kg_col": [128, 1], "bt": [128, 8, 3, 256],
    "c31": [128, 8], "vmask": [128, 64], "e8": [8, 1024], "rg_bc": [128, 2048],
    "cosT": [128, 2048], "sinT": [128, 2048], "decT": [128, 8, 128], "qdec": [128, 8, 128],
    "kdec": [128, 8], "flag": [128, 1], "w_attn_br": [1024, 2048], "w_ret_br": [2048, 2048],
    "w_o": [2048, 2048], "w_up": [2048, 11264], "cw": [128, 88, 3], "cb": [128, 88],
    "w_down": [5632, 2048], "ident": [128, 128],
}


def build_program(stop_after=None, debug=()):
    nc = bass.Bass("TRN2", target_bir_lowering=False)
    class _LazyIn(dict):
        def __missing__(self, k):
            self[k] = nc.dram_tensor(k, INPUT_SHAPES[k], F32, kind="ExternalInput").ap()
            return self[k]
    din = _LazyIn()
    y = nc.dram_tensor("y", [OWN, D], F32, kind="ExternalOutput").ap()
    x1s = nc.dram_tensor("x1s", [OWN, D], F32, kind="Internal").ap()
    zTs = nc.dram_tensor("zTs", [16, 128, OWN], BF16, kind="Internal").ap()
    dbg_out = {}
    cd = _const_tables(1)["chunk_decay"]
    X1KEYS = [("x1s", d) for d in range(8)]

    with contextlib.ExitStack() as st:
        slab_t = st.enter_context(nc.sbuf_tensor("slab", [128, 4, NKC, 256], BF16))
        hT_t = st.enter_context(nc.sbuf_tensor("hT", [128, 32768], BF16))
        A_t = st.enter_context(nc.sbuf_tensor("A", [128, 49152], BF16))
        C_t = st.enter_context(nc.sbuf_tensor("C", [128, 4096], BF16))
        psb = [st.enter_context(nc.psum_tensor("ps%d" % i, [128, 512], F32)) for i in range(8)]
        S = Sched(nc)

        def carve(t, boff, dt, shape):
            n = 1
            for s in shape[1:]:
                n *= s
            nb = n * _esz(dt)
            v = t[:, boff // 2:(boff + nb) // 2]
            if dt == F32:
                v = v.bitcast(F32)
            if len(shape) == 3:
                v = v.rearrange("p (a b) -> p a b", a=shape[1])
            elif len(shape) == 4:
                v = v.rearrange("p (a b c) -> p a b c", a=shape[1], b=shape[2])
            if shape[0] != 128:
                v = v[0:shape[0]]
            return v

        class Arena:
            def __init__(self, t, size):
                self.t, self.size, self.off = t, size, 0

            def alloc(self, dt, shape):
                self.off = (self.off + 63) // 64 * 64
                v = carve(self.t, self.off, dt, shape)
                n = _esz(dt)
                for s in shape[1:]:
                    n *= s
                self.off += n
                assert self.off <= self.size, (self.off, self.size)
                return v

        CA = Arena(C_t, 8192)
        AA = Arena(A_t, 98304)
        HA = Arena(hT_t, 65536)

        ident32 = CA.alloc(F32, [128, 128])
        ident_bf = CA.alloc(BF16, [128, 128])
        ones_bf = CA.alloc(BF16, [128, 128])
        ccol = CA.alloc(F32, [128, 16])
        s_bf = CA.alloc(BF16, [128, 16])
        s_bf2 = CA.alloc(BF16, [128, 16, 2])
        bcol = CA.alloc(F32, [128, 96])
        modc = CA.alloc(F32, [128, 4, 16])
        g1c = CA.alloc(F32, [128, 16])
        g2c = CA.alloc(F32, [128, 16])
        A1c = CA.alloc(F32, [128, 16])
        A2c = CA.alloc(F32, [128, 16])
        qgc = CA.alloc(F32, [128, 1])
        kgc = CA.alloc(F32, [128, 1])
        c31 = CA.alloc(F32, [128, 8])
        vmask = CA.alloc(F32, [128, 64])
        kdec = CA.alloc(F32, [128, 8])
        flag = CA.alloc(F32, [128, 1])
        epsc = CA.alloc(F32, [128, 1])
        ssq = CA.alloc(F32, [128, 17])
        rstd = CA.alloc(F32, [128, 17])
        smallf = CA.alloc(F32, [128, 8])
        km = CA.alloc(F32, [128, 8])
        km_bf = CA.alloc(BF16, [128, 8])
        gm = CA.alloc(F32, [128, 64])
        mx8 = CA.alloc(F32, [128, 64])
        nm = CA.alloc(F32, [128, 64])
        nm_bf = CA.alloc(BF16, [128, 64])
        bnst = CA.alloc(F32, [128, 3, 6])
        bnmv = CA.alloc(F32, [128, 3, 2])
        qnh = CA.alloc(BF16, [128, 4, 2])
        yaTh = CA.alloc(BF16, [128, 8, 2])
        qrTh = CA.alloc(BF16, [128, 2, 2])
        sTh = CA.alloc(BF16, [128, 2])
        qdTh = CA.alloc(BF16, [128, 2])
        zTh = CA.alloc(BF16, [128, 16, 2])
        mTh = CA.alloc(BF16, [128, 16, 2])
        h2Th = CA.alloc(BF16, [128, 16, 2])
        Phs = CA.alloc(F32, [128, 2, 2])

        def dma_sp(out, in_, reads, writes, key):
            S.op("sp", lambda e: e.dma_start(out=out, in_=in_), reads=reads, writes=writes, dma=key)

        def dma_pool(out, in_, reads, writes, key):
            S.op("pool", lambda e: e.dma_start(out=out, in_=in_), reads=reads, writes=writes, dma=key)

        def load_const(dst, name, cast=False):
            if cast:
                dma_pool(dst, din[name], [], [dst], "c_" + name + "_c")
            else:
                dma_sp(dst, din[name], [], [dst], "c_" + name)

        def mm(out, lhsT, rhs, start, stop):
            S.op("pe", lambda e: e.matmul(out, lhsT, rhs, start=start, stop=stop),
                 reads=[lhsT, rhs], writes=[out])

        def tp(out, in_, ident):
            S.op("pe", lambda e: e.matmul(out, in_, ident, start=True, stop=True), reads=[in_, ident], writes=[out])

        def act(out, in_, func, bias=None, scale=None, accum=None):
            kw = {}
            r = [in_]
            w = [out]
            if bias is not None:
                kw["bias"] = bias
                if not isinstance(bias, float):
                    r.append(bias)
            if scale is not None:
                kw["scale"] = scale
                if not isinstance(scale, float):
                    r.append(scale)
            if accum is not None:
                kw["accum_out"] = accum
                w.append(accum)
            S.op("act", lambda e: e.activation(out, in_, func, **kw), reads=r, writes=w)

        def ts(out, in0, s1, s2, op0, op1=None):
            r = [in0]
            if not isinstance(s1, float):
                r.append(s1)
            if s2 is not None and not isinstance(s2, float):
                r.append(s2)
            if op1 is None:
                S.op("dve", lambda e: e.tensor_scalar(out, in0, s1, None, op0), reads=r, writes=[out])
            else:
                S.op("dve", lambda e: e.tensor_scalar(out, in0, s1, s2, op0, op1), reads=r, writes=[out])

        def tt(out, in0, in1, op):
            S.op("dve", lambda e: e.tensor_tensor(out, in0, in1, op), reads=[in0, in1], writes=[out])

        def stt(out, in0, sc, in1, op0, op1):
            r = [in0, in1]
            if not isinstance(sc, float):
                r.append(sc)
            S.op("dve", lambda e: e.scalar_tensor_tensor(out, in0, sc, in1, op0, op1), reads=r, writes=[out])

        def recip(out, in_):
            S.op("dve", lambda e: e.reciprocal(out, in_), reads=[in_], writes=[out])

        def memset(ap, v):
            S.op("dve", lambda e: e.memset(ap, v), reads=[], writes=[ap])

        def acopy(out, in_):
            S.op("act", lambda e: e.copy(out, in_), reads=[in_], writes=[out])

        def vcopy(out, in_):
            S.op("dve", lambda e: e.tensor_copy(out, in_), reads=[in_], writes=[out])

        ev_tog = [0]

        def evac_affine(out, in_, sc, bi):
            ts(out, in_, sc, bi, ALU.mult, ALU.add)

        def evac_copy(out, in_):
            vcopy(out, in_)

        bank_ctr = [0]

        def bank():
            b = bank_ctr[0] % 8
            bank_ctr[0] += 1
            return psb[b][:]

        slab_ctr = [0]

        def slab_load(w, r0, nk, c0, ncols=256):
            i = slab_ctr[0] % 4
            slab_ctr[0] += 1
            dst = slab_t[:, i, 0:nk, 0:ncols]
            src = w[r0:r0 + nk * 128, c0:c0 + ncols].rearrange("(kc p) n -> p kc n", p=128)
            dma_pool(dst, src, [], [slab_t[:, i, :, :]], ("slab", i))
            return slab_t[:, i]

        def dump(name, ap, shape, dt):
            if name not in debug:
                return
            o = nc.dram_tensor("dbg_" + name, list(shape), dt, kind="ExternalOutput").ap()
            dbg_out[name] = o
            dma_sp(o, ap, [ap], ["dbg_" + name], "dbg_" + name)

        def finish(extra=()):
            fw = [k for k in extra] + ["dbg_" + k for k in dbg_out]
            S.emit(final_wait=fw)
            return nc, dbg_out

        load_const(ident32, "ident")
        load_const(ident_bf, "ident", cast=True)
        load_const(ccol, "c_col")
        load_const(bcol, "b_col")
        load_const(g1c, "g1_col")
        load_const(g2c, "g2_col")
        load_const(qgc, "qg_col")
        load_const(kgc, "kg_col")
        load_const(c31, "c31")
        load_const(vmask, "vmask")
        load_const(kdec, "kdec")
        load_const(flag, "flag")
        memset(ones_bf, 1.0)
        memset(epsc, EPS)
        act(s_bf, ccol, AF.Silu)
        vcopy(s_bf2[:, :, 0], s_bf)
        vcopy(s_bf2[:, :, 1], s_bf)

        def build_s_rep(s_rep):
            for kc in range(NKC):
                ts(s_rep[:, kc, :], ones_bf, s_bf[:, kc:kc + 1], None, ALU.mult)

        if stop_after == "0":
            dump("s_bf", s_bf, [128, 16], BF16)
            return finish()
        def mod_col_section(sec_in_w, sec_out):
            ps = bank()
            for j in range(8):
                sl = slab_load(din["w_ada"], 0, NKC, sec_in_w * 2048 + j * 256)
                for m in range(2):
                    col = j * 2 + m
                    for kc in range(NKC):
                        mm(ps[:, 2 * col:2 * col + 2], sl[:, kc, m * 128:(m + 1) * 128], s_bf2[:, kc, :],
                           kc == 0, kc == NKC - 1)
            tt(modc[:, sec_out, :], ps[:, 0:32].rearrange("p (a b) -> p a b", b=2)[:, :, 0],
               bcol[:, sec_in_w * 16:(sec_in_w + 1) * 16], ALU.add)

        def mod_col_slab(sec_in_w, sec_out, j):
            sl = slab_load(din["w_ada"], 0, NKC, sec_in_w * 2048 + j * 256)
            ps = bank()
            for m in range(2):
                for kc in range(NKC):
                    mm(ps[:, 2 * m:2 * m + 2], sl[:, kc, m * 128:(m + 1) * 128], s_bf2[:, kc, :],
                       kc == 0, kc == NKC - 1)
            tt(modc[:, sec_out, 2 * j:2 * j + 2], ps[:, 0:4].rearrange("p (a b) -> p a b", b=2)[:, :, 0],
               bcol[:, sec_in_w * 16 + 2 * j:sec_in_w * 16 + 2 * j + 2], ALU.add)

        def mod_row_slab(sec_in_w, dst, s_rep, j):
            sl = slab_load(din["w_ada"], 0, NKC, sec_in_w * 2048 + j * 256)
            ps = bank()
            for kc in range(NKC):
                mm(ps[:, 0:256], s_rep[:, kc, :], sl[:, kc, :], kc == 0, kc == NKC - 1)
            evac_copy(dst[:, j * 256:(j + 1) * 256], ps[:, 0:256])

        def mod_row_section(sec_in_w, dst, s_rep):
            for j in range(8):
                sl = slab_load(din["w_ada"], 0, NKC, sec_in_w * 2048 + j * 256)
                ps = bank()
                for kc in range(NKC):
                    mm(ps[:, 0:256], s_rep[:, kc, :], sl[:, kc, :], kc == 0, kc == NKC - 1)
                evac_copy(dst[:, j * 256:(j + 1) * 256], ps[:, 0:256])

        import os as _os
        if _os.environ.get("SKIPA"):
            memset(modc[:, 0:2, :], 0.5)
        else:
            mod_col_section(1, 1)
            mod_col_section(0, 0)
        ts(A1c, modc[:, 1, :], 1.0, None, ALU.add)
        tt(A1c, A1c, g1c, ALU.mult)
        B1c = modc[:, 0, :]
        dump("modc", modc[:, 0:2, :], [128, 2, 16], F32)
        if stop_after == "A":
            return finish()

        hT = carve(hT_t, 0, BF16, [128, 2, NKC, 1024])

        def norm_tile(xt, junk, np_, si, Ac, Bc, dst_fn):
            act(junk[0:np_, :], xt, AF.Square, accum=ssq[0:np_, si:si + 1])
            act(rstd[0:np_, si:si + 1], ssq[0:np_, si:si + 1], AF.Sqrt, bias=epsc[0:np_, :], scale=1.0 / D)
            recip(rstd[0:np_, si:si + 1], rstd[0:np_, si:si + 1])
            ts(junk[0:np_, :], xt, rstd[0:np_, si:si + 1], None, ALU.mult)
            import os as _os
            if _os.environ.get("NB") == "1":
                dump("junk", junk, [128, 2048], BF16)
                return
            for q4 in range(int(_os.environ.get("NQ", "4"))):
                ps = bank()
                for j in range(4):
                    dc = q4 * 4 + j
                    tp(ps[:, j * 128:j * 128 + np_], junk[0:np_, dc * 128:(dc + 1) * 128], ident_bf[0:np_, 0:np_])
                for j in range(4):
                    dc = q4 * 4 + j
                    evac_affine(dst_fn(dc), ps[:, j * 128:j * 128 + np_], Ac[:, dc:dc + 1], Bc[:, dc:dc + 1])

        AA.off = 0
        xt_slots = [AA.alloc(F32, [128, 2048]) for _ in range(2)]
        junk = AA.alloc(BF16, [128, 2048])
        import os as _os
        for t in range(int(_os.environ.get("NT", "16"))):
            xt = xt_slots[t % 2]
            dma_sp(xt, din["xw"][t * 128:(t + 1) * 128, :], [], [xt], ("xt", t % 2))
            norm_tile(xt, junk, 128, t, A1c, B1c,
                      lambda dc, t=t: hT[:, t // 8, dc, (t % 8) * 128:(t % 8 + 1) * 128])
        dump("hT", hT_t[:, :], [128, 32768], BF16)
        dump("hTs", hT_t[:, 0:1024], [128, 1024], BF16)
        if stop_after == "B":
            return finish()

        def h_tok(qd):
            return lambda kc: hT[:, qd // 2, kc, (qd % 2) * 512:(qd % 2) * 512 + 512]

        def h_halo(kc):
            return hT[:, 0, kc, 1022:1024]

        def h_tile(t):
            return lambda kc: hT[:, t // 8, kc, (t % 8) * 128:(t % 8 + 1) * 128]

        def proj_fm(sl, m, rhs_fn, w=512, nk=NKC):
            ps = bank()
            for kc in range(nk):
                mm(ps[:, 0:w], sl[:, kc, m * 128:(m + 1) * 128], rhs_fn(kc), kc == 0, kc == nk - 1)
            return ps

        def proj_tm(sl, tiles, dst_fn, post=None, after_first=None):
            for i in range(0, len(tiles), 2):
                if i == 2 and after_first is not None:
                    after_first()
                ps = bank()
                for u in range(2):
                    lf = h_tile(tiles[i + u])
                    for kc in range(NKC):
                        mm(ps[:, u * 256:(u + 1) * 256], lf(kc), sl[:, kc, :], kc == 0, kc == NKC - 1)
                src = ps[:].rearrange("p (a b) -> p a b", a=2)
                if post is None:
                    evac_copy(dst_fn(i), src)
                else:
                    post(dst_fn(i), src)

        AA.off = 0
        qn = AA.alloc(BF16, [128, 4, 1024])
        kn = AA.alloc(BF16, [128, 4, 2048])
        va = AA.alloc(BF16, [128, 16, 512])
        R_END = AA.off
        yaT = AA.alloc(BF16, [128, 8, 1024])
        Y_END = AA.off
        BT = AA.alloc(F32, [128, 4, 3, 256])
        sq = [AA.alloc(BF16, [128, 512]) for _ in range(2)]
        f32a = [AA.alloc(F32, [128, 512]) for _ in range(2)]
        f32b = [AA.alloc(F32, [128, 512]) for _ in range(2)]
        pT = [AA.alloc(BF16, [128, 256]) for _ in range(3)]
        etmp = [AA.alloc(F32, [128, 256]) for _ in range(3)]
        rinv = AA.alloc(F32, [128, 256])
        e8 = AA.alloc(BF16, [8, 1024])
        nmT2 = [AA.alloc(BF16, [8, 1024]) for _ in range(2)]
        qf32 = [AA.alloc(F32, [128, 512]) for _ in range(2)]
        load_const(e8, "e8", cast=True)
        tctr = [0]

        qk_pend = []

        def qknorm_flush():
            while qk_pend:
                i, qf, gcol, out, w = qk_pend.pop(0)
                ps2 = bank()
                mm(ps2[:, 0:w], ones_bf, sq[i][:, 0:w], True, True)
                ts(f32a[i][:, 0:w], ps2[:, 0:w], 1.0 / 128, EPS, ALU.mult, ALU.add)
                act(f32a[i][:, 0:w], f32a[i][:, 0:w], AF.Sqrt)
                recip(f32b[i][:, 0:w], f32a[i][:, 0:w])
                stt(out, qf, gcol, f32b[i][:, 0:w], ALU.mult, ALU.mult)

        def qknorm(ps, gcol, out, w=512):
            i = tctr[0] % 2
            tctr[0] += 1
            qf = qf32[i][:, 0:w]
            vcopy(qf, ps[:, 0:w])
            act(sq[i][:, 0:w], qf, AF.Square)
            qknorm_flush()
            qk_pend.append((i, qf, gcol, out, w))

        def gating(hl):
            nmT = nmT2[hl % 2]
            v3 = kn[:, hl, :].rearrange("p (a b) -> p a b", a=8)
            S.op("dve", lambda e: e.tensor_reduce(km, v3, AX.X, ALU.add), reads=[kn[:, hl, :]], writes=[km])
            ts(km_bf, km, 1.0 / 256, None, ALU.mult)
            psg = psb[7]
            for qt in range(8):
                mm(psg[:, qt * 8:(qt + 1) * 8], qn[:, hl, qt * 128:(qt + 1) * 128], km_bf, True, True)
            tt(gm, psg[:, 0:64], vmask, ALU.add)
            for qt in range(8):
                S.op("dve", lambda e, qt=qt: e.max(mx8[:, qt * 8:(qt + 1) * 8], gm[:, qt * 8:(qt + 1) * 8]),
                     reads=[gm], writes=[mx8[:, qt * 8:(qt + 1) * 8]])
            for qt in range(8):
                ts(nm[:, qt * 8:(qt + 1) * 8], gm[:, qt * 8:(qt + 1) * 8],
                   mx8[:, qt * 8 + 2:qt * 8 + 3], NEG, ALU.is_lt, ALU.mult)
            tt(nm_bf, nm, vmask, ALU.add)
            pst = psb[7][:]
            for hq in range(2):
                for u in range(4):
                    qt = hq * 4 + u
                    tp(pst[0:8, u * 128:(u + 1) * 128], nm_bf[:, qt * 8:(qt + 1) * 8], ident_bf)
                vcopy(nmT[0:8, hq * 512:(hq + 1) * 512], pst[0:8, :])

        lctr = [0]

        def attn_block(hl, h, J, qs, w, btc0, mask_ap, psO, psS, out):
            kts = list(range(2 * (J + 1)))

            def qk(kt):
                n = kt // 2
                psL = psb[4 + lctr[0] % 3]
                pt = pT[lctr[0] % 3]
                lctr[0] += 1
                own = (n == J)
                use_mask = (not own) and (mask_ap is not None)
                mm(psL[:, 0:w], kn[:, hl, kt * 128:(kt + 1) * 128], qs, True, not use_mask)
                if use_mask:
                    mm(psL[:, 0:w], e8[0:8, n * 128:(n + 1) * 128], mask_ap, False, True)
                bti = None
                if own:
                    bti = kt - 2 * J
                elif n == J - 1 and kt % 2 == 1:
                    bti = 2
                et = etmp[lctr[0] % 3]
                if bti is None:
                    ts(et[:, 0:w], psL[:, 0:w], ATT_SCALE, c31[:, h:h + 1], ALU.mult, ALU.add)
                else:
                    stt(et[:, 0:w], psL[:, 0:w], ATT_SCALE, BT[:, hl, bti, btc0:btc0 + w], ALU.mult, ALU.add)
                act(pt[:, 0:w], et[:, 0:w], AF.Exp)
                return pt

            def pv(kt, pt, first, last):
                mm(psO[:, 0:w], va[:, kt, hl * 128:(hl + 1) * 128], pt[:, 0:w], first, last)
                mm(psS[:, 0:w], ones_bf, pt[:, 0:w], first, last)

            pend = []
            for kt in kts:
                pend.append((kt, qk(kt)))
                if len(pend) > 2:
                    k0, p0 = pend.pop(0)
                    pv(k0, p0, k0 == kts[0], False)
            while pend:
                k0, p0 = pend.pop(0)
                pv(k0, p0, k0 == kts[0], len(pend) == 0)
            recip(rinv[:, 0:w], psS[:, 0:w])
            tt(out, psO[:, 0:w], rinv[:, 0:w], ALU.mult)

        def attention(hl, h):
            for jq in range(4):
                attn_block(hl, h, 4 + jq, qn[:, hl, jq * 256:(jq + 1) * 256], 256, 0,
                           nmT2[hl % 2][0:8, jq * 256:(jq + 1) * 256], psb[jq % 2], psb[2 + jq % 2],
                           yaT[:, h, jq * 256:(jq + 1) * 256])
            attn_block(hl, h, 3, qnh[:, hl, :], 2, 254, None, psb[0], psb[2], yaTh[:, h, :])

        for g in range(2):
            dma_sp(BT, din["bt"][:, 4 * g:4 * g + 4], [], [BT], "bt")
            for s in range(2):
                sl = slab_load(din["w_in"], 0, NKC, g * 512 + s * 256)
                for m in range(2):
                    for th in range(2):
                        ps = proj_fm(sl, m, h_tok(2 + th))
                        qknorm(ps, qgc[:, 0:1], qn[:, 2 * s + m, th * 512:(th + 1) * 512])
                    ps = proj_fm(sl, m, h_halo, w=2)
                    qknorm(ps, qgc[:, 0:1], qnh[:, 2 * s + m, :], w=2)
            for s in range(2):
                sl = slab_load(din["w_in"], 0, NKC, 1024 + g * 512 + s * 256)
                for m in range(2):
                    for qd in range(4):
                        ps = proj_fm(sl, m, h_tok(qd))
                        qknorm(ps, kgc[:, 0:1], kn[:, 2 * s + m, qd * 512:(qd + 1) * 512])
            for s in range(2):
                sl = slab_load(din["w_in"], 0, NKC, 2048 + g * 512 + s * 256)
                proj_tm(sl, list(range(16)), lambda i, s=s: va[:, i:i + 2, s * 256:(s + 1) * 256],
                        after_first=qknorm_flush)
            qknorm_flush()
            if g == 0:
                dump("qn", qn, [128, 4, 1024], BF16)
                dump("kn", kn, [128, 4, 2048], BF16)
                dump("va", va, [128, 16, 512], BF16)
            gating(0)
            if g == 0:
                dump("nmT", nmT2[0], [8, 1024], BF16)
            for hl in range(4):
                if hl < 3:
                    gating(hl + 1)
                attention(hl, 4 * g + hl)
        dump("yaT", yaT, [128, 8, 1024], BF16)
        dump("yaTh", yaTh, [128, 8, 2], BF16)
        if stop_after == "C":
            return finish()

        AA.off = 0
        qrT = AA.alloc(BF16, [128, 2, 1024])
        krT = AA.alloc(BF16, [128, 2, 2048])
        kdT = AA.alloc(BF16, [128, 2, 16, 128])
        vr = AA.alloc(BF16, [128, 16, 256])
        Sb = AA.alloc(BF16, [128, 8, 256])
        sT = AA.alloc(BF16, [128, 8, 128])
        qdT = AA.alloc(BF16, [128, 8, 128])
        assert AA.off <= R_END, AA.off
        AA.off = Y_END
        zt = AA.alloc(BF16, [128, 8, 512])
        sg = AA.alloc(BF16, [128, 8, 256])
        zst = AA.alloc(BF16, [128, 2, 1024])
        sgh = AA.alloc(BF16, [128, 256])
        Sbh = AA.alloc(BF16, [128, 256])
        zth = AA.alloc(BF16, [128, 512])
        ynh = AA.alloc(F32, [128, 256])
        cs = AA.alloc(F32, [128, 2, 512])
        rgb = AA.alloc(F32, [128, 512])
        decTh = AA.alloc(F32, [128, 128])
        qdech = AA.alloc(F32, [128, 128])
        Sst = AA.alloc(F32, [128, 256])
        r32a = [AA.alloc(F32, [128, 512]) for _ in range(2)]
        r32b = [AA.alloc(F32, [128, 512]) for _ in range(2)]
        yn = [AA.alloc(F32, [128, 256]) for _ in range(2)]
        sgtmp = AA.alloc(F32, [128, 2, 256])
        rctr = [0]

        def rotary(ps, out, c0=0, w=512):
            i = rctr[0] % 2
            rctr[0] += 1
            a, b2 = r32a[i], r32b[i]
            tt(a[:, 0:w], ps[:, 0:w], cs[:, 0, c0:c0 + w], ALU.mult)
            tt(b2[0:64, 0:w], ps[64:128, 0:w], cs[64:128, 1, c0:c0 + w], ALU.mult)
            tt(b2[64:128, 0:w], ps[0:64, 0:w], cs[0:64, 1, c0:c0 + w], ALU.mult)
            tt(out, a[:, 0:w], b2[:, 0:w], ALU.add)

        def groupnorm_gate(o, np_, bi, gslice, sgv, ytmp, dst):
            S.op("dve", lambda e: e.bn_stats(bnst[0:np_, bi, :], o), reads=[o], writes=[bnst[0:np_, bi, :]])
            S.op("dve", lambda e: e.bn_aggr(bnmv[0:np_, bi, :], bnst[0:np_, bi, :]),
                 reads=[bnst[0:np_, bi, :]], writes=[bnmv[0:np_, bi, :]])
            act(smallf[0:np_, bi:bi + 1], bnmv[0:np_, bi, 1:2], AF.Sqrt, bias=epsc[0:np_, :], scale=1.0)
            recip(smallf[0:np_, bi:bi + 1], smallf[0:np_, bi:bi + 1])
            ts(ytmp, o, bnmv[0:np_, bi, 0:1], smallf[0:np_, bi:bi + 1], ALU.subtract, ALU.mult)
            tt(ytmp, ytmp, gslice, ALU.mult)
            tt(dst, ytmp, sgv, ALU.mult)

        for rg in range(4):
            slq = slab_load(din["w_in"], 0, NKC, 3072 + rg * 256)
            slk = slab_load(din["w_in"], 0, NKC, 4096 + rg * 256)
            dma_sp(rgb, din["rg_bc"][:, rg * 512:(rg + 1) * 512], [], [rgb], "rgb")
            for qd in range(4):
                dma_sp(cs[:, 0, :], din["cosT"][:, qd * 512:(qd + 1) * 512], [], [cs[:, 0, :]], "cs0")
                dma_sp(cs[:, 1, :], din["sinT"][:, qd * 512:(qd + 1) * 512], [], [cs[:, 1, :]], "cs1")
                for m in range(2):
                    ps = proj_fm(slk, m, h_tok(qd))
                    rotary(ps, krT[:, m, qd * 512:(qd + 1) * 512])
                if qd == 1:
                    for m in range(2):
                        ps = proj_fm(slq, m, h_halo, w=2)
                        rotary(ps, qrTh[:, m, :], c0=510, w=2)
                if qd >= 2:
                    for m in range(2):
                        ps = proj_fm(slq, m, h_tok(qd))
                        rotary(ps, qrT[:, m, (qd - 2) * 512:(qd - 1) * 512])
            if rg == 0:
                dump("qrT", qrT, [128, 2, 1024], BF16)
                dump("krT", krT, [128, 2, 2048], BF16)
            for m in range(2):
                h = 2 * rg + m
                for qq in range(4):
                    pst = bank()
                    for u in range(4):
                        t = qq * 4 + u
                        tp(pst[:, u * 128:(u + 1) * 128], krT[:, m, t * 128:(t + 1) * 128], ident_bf)
                    dst = kdT[:, m, qq * 4:(qq + 1) * 4, :]
                    src = pst.rearrange("p (a b) -> p a b", a=4)
                    ts(dst, src, kdec[:, h:h + 1], None, ALU.mult)
            for m in range(2):
                h = 2 * rg + m
                dma_sp(decTh, din["decT"][:, h, :], [], [decTh], "decTh")
                dma_sp(qdech, din["qdec"][:, h, :], [], [qdech], "qdech")
                slv = slab_load(din["w_in"], 0, NKC, 5120 + h * 256)
                proj_tm(slv, list(range(16)), lambda i: vr[:, i:i + 2, :])
                for n in range(15):
                    o = bank()[:, 0:256]
                    mm(o, kdT[:, m, n, :], vr[:, n, :], True, True)
                    if n == 0:
                        vcopy(Sst, o)
                    else:
                        if n == 7:
                            acopy(Sbh, Sst)
                        if n == 8:
                            ts(Sst, Sst, flag[:, 0:1], None, ALU.mult)
                        if n >= 8:
                            acopy(Sb[:, n - 8, :], Sst)
                        stt(Sst, Sst, cd[h], o, ALU.mult, ALU.add)
                acopy(Sb[:, 7, :], Sst)
                for c4 in range(2):
                    pss = bank()
                    for u in range(4):
                        c = c4 * 4 + u
                        mm(pss[:, u * 128:(u + 1) * 128], krT[:, m, (8 + c) * 128:(9 + c) * 128],
                           qrT[:, m, c * 128:(c + 1) * 128], True, True)
                    for u in range(4):
                        c = c4 * 4 + u
                        tt(sT[:, c, :], pss[:, u * 128:(u + 1) * 128], decTh, ALU.mult)
                for c in range(8):
                    tt(qdT[:, c, :], qrT[:, m, c * 128:(c + 1) * 128], qdech, ALU.mult)
                slg = slab_load(din["w_in"], 0, NKC, 7168 + h * 256)
                def _silu_post(d, s_):
                    vcopy(sgtmp, s_)
                    act(d, sgtmp, AF.Silu)
                proj_tm(slg, list(range(8, 16)), lambda i: sg[:, i:i + 2, :], post=_silu_post)
                psh = bank()
                for kc in range(NKC):
                    mm(psh[0:2, 0:256], h_halo(kc), slg[:, kc, :], kc == 0, kc == NKC - 1)
                vcopy(ynh[0:2, :], psh[0:2, 0:256])
                act(sgh[0:2, :], ynh[0:2, :], AF.Silu)
                for c in range(8):
                    o = bank()[:, 0:256]
                    mm(o, sT[:, c, :], vr[:, 8 + c, :], True, False)
                    mm(o, qdT[:, c, :], Sb[:, c, :], False, True)
                    groupnorm_gate(o, 128, c % 2, rgb[:, m * 256:(m + 1) * 256], sg[:, c, :], yn[c % 2],
                                   zt[:, c, m * 256:(m + 1) * 256])
                pss = bank()
                mm(pss[:, 0:2], krT[:, m, 7 * 128:8 * 128], qrTh[:, m, :], True, True)
                tt(sTh, pss[:, 0:2], decTh[:, 126:128], ALU.mult)
                tt(qdTh, qrTh[:, m, :], qdech[:, 126:128], ALU.mult)
                psy = bank()
                mm(psy[0:2, 0:256], sTh, vr[:, 7, :], True, False)
                mm(psy[0:2, 0:256], qdTh, Sbh, False, True)
                groupnorm_gate(psy[0:2, 0:256], 2, 2, rgb[0:2, m * 256:(m + 1) * 256], sgh[0:2, :],
                               ynh[0:2, :], zth[0:2, m * 256:(m + 1) * 256])
            if rg == 0:
                dump("zt", zt, [128, 8, 512], BF16)
            for fc in range(4):
                for hc in range(2):
                    pst = bank()
                    for u in range(4):
                        c = hc * 4 + u
                        tp(pst[:, u * 128:(u + 1) * 128], zt[:, c, fc * 128:(fc + 1) * 128], ident_bf)
                    evac_copy(zst[:, fc % 2, hc * 512:(hc + 1) * 512], pst)
                if fc % 2 == 1:
                    a0 = rg * 4 + fc - 1
                    dma_sp(zTs[a0:a0 + 2].rearrange("a p t -> p a t"), zst, [zst], ["zTs"], "zst")
            psth = bank()
            for fc in range(4):
                tp(psth[:, fc * 2:fc * 2 + 2], zth[0:2, fc * 128:(fc + 1) * 128], ident_bf[0:2, 0:2])
            vcopy(zTh[:, rg * 4:(rg + 1) * 4, :], psth[:, 0:8].rearrange("p (a b) -> p a b", a=4))
        dump("zTh", zTh, [128, 16, 2], BF16)
        if stop_after == "D":
            return finish()

        zT = carve(hT_t, 0, BF16, [128, 16, 1024])
        hTh = CA.alloc(BF16, [128, 16, 2])
        vcopy(hTh, hT[:, 0, :, 1022:1024])
        dma_sp(zT, zTs.rearrange("a p t -> p a t"), ["zTs"], [zT], "zTl")
        AA.off = Y_END
        mT = AA.alloc(BF16, [128, 16, 1024])
        AA.off = 0
        g1bc = AA.alloc(F32, [128, 2048])
        s_rep = AA.alloc(BF16, [128, 16, 128])
        F_BASE = AA.off
        build_s_rep(s_rep)
        gA = [AA.alloc(F32, [128, 512]) for _ in range(4)]
        gB = [AA.alloc(F32, [128, 512]) for _ in range(4)]
        gAh = AA.alloc(F32, [128, 2, 2])
        gBh = AA.alloc(F32, [128, 2, 2])
        assert AA.off <= R_END
        grp = [(m, th) for m in range(2) for th in range(2)]

        def gate_pass(sl, dstl, dsth):
            for gi, (m, th) in enumerate(grp):
                p = proj_fm(sl, m, h_tok(2 + th))
                vcopy(dstl[gi], p)
                act(dstl[gi], dstl[gi], AF.Sigmoid)
            for m in range(2):
                p = proj_fm(sl, m, lambda kc: hTh[:, kc, :], w=2)
                vcopy(dsth[:, m, :], p[:, 0:2])
                act(dsth[:, m, :], dsth[:, m, :], AF.Sigmoid)

        for dg in range(8):
            slga = slab_load(din["w_in"], 0, NKC, 9216 + dg * 256)
            gate_pass(slga, gA, gAh)
            slgb = slab_load(din["w_in"], 0, NKC, 11264 + dg * 256)
            gate_pass(slgb, gB, gBh)
            sla = slab_load(din["w_attn_br"], 0, 8, dg * 256)
            for gi, (m, th) in enumerate(grp):
                tsl = slice(th * 512, (th + 1) * 512)
                p = proj_fm(sla, m, lambda kc, tsl=tsl: yaT[:, kc, tsl], nk=8)
                tt(gA[gi], p, gA[gi], ALU.mult)
            for m in range(2):
                p = proj_fm(sla, m, lambda kc: yaTh[:, kc, :], w=2, nk=8)
                tt(gAh[:, m, :], p[:, 0:2], gAh[:, m, :], ALU.mult)
            mod_row_slab(2, g1bc, s_rep, dg)
            slr = slab_load(din["w_ret_br"], 0, NKC, dg * 256)
            for gi, (m, th) in enumerate(grp):
                tsl = slice(th * 512, (th + 1) * 512)
                p = proj_fm(slr, m, lambda kc, tsl=tsl: zT[:, kc, tsl])
                tt(gB[gi], p, gB[gi], ALU.mult)
                tt(mT[:, dg * 2 + m, tsl], gA[gi], gB[gi], ALU.add)
            for m in range(2):
                p = proj_fm(slr, m, lambda kc: zTh[:, kc, :], w=2)
                tt(gBh[:, m, :], p[:, 0:2], gBh[:, m, :], ALU.mult)
                tt(mTh[:, dg * 2 + m, :], gAh[:, m, :], gBh[:, m, :], ALU.add)
        dump("mT", mT, [128, 16, 1024], BF16)
        dump("mTh", mTh, [128, 16, 2], BF16)
        if stop_after == "E":
            return finish()

        AA.off = F_BASE
        btmp = AA.alloc(F32, [128, 2048])
        xin = [AA.alloc(F32, [128, 8, 256]) for _ in range(2)]
        xh = AA.alloc(F32, [128, 2048])
        assert AA.off <= Y_END
        dma_sp(btmp, din["b_g1"], [], [btmp], "btmp")
        dma_sp(xh[0:2, :], din["xw"][1022:1024, :], [], [xh[0:2, :]], "xh")
        assert AA.off <= Y_END
        tt(g1bc, g1bc, btmp, ALU.add)
        xown = din["xw"][1024:2048, :]
        for dg in range(8):
            dsl = slice(dg * 256, (dg + 1) * 256)
            sl = slab_load(din["w_o"], 0, NKC, dg * 256)
            xi = xin[dg % 2]
            dma_sp(xi, xown[:, dsl].rearrange("(t p) n -> p t n", p=128), [], [xi], ("xin", dg % 2))
            for t2 in range(4):
                ps = bank()
                for u in range(2):
                    t = t2 * 2 + u
                    for kc in range(NKC):
                        mm(ps[:, u * 256:(u + 1) * 256], mT[:, kc, t * 128:(t + 1) * 128], sl[:, kc, :],
                           kc == 0, kc == NKC - 1)
                for u in range(2):
                    tt(ps[:, u * 256:(u + 1) * 256], ps[:, u * 256:(u + 1) * 256], g1bc[:, dsl], ALU.mult)
                src = ps[:].rearrange("p (a b) -> p a b", a=2)
                tt(xi[:, t2 * 2:t2 * 2 + 2, :], xi[:, t2 * 2:t2 * 2 + 2, :], src, ALU.add)
            ps = bank()
            for kc in range(NKC):
                mm(ps[0:2, 0:256], mTh[:, kc, :], sl[:, kc, :], kc == 0, kc == NKC - 1)
            tt(ps[0:2, 0:256], ps[0:2, 0:256], g1bc[0:2, dsl], ALU.mult)
            tt(xh[0:2, dsl], xh[0:2, dsl], ps[0:2, 0:256], ALU.add)
            dma_sp(x1s[:, dsl].rearrange("(t p) n -> p t n", p=128), xi, [xi], [("x1s", dg)], ("x1w", dg % 2))
            mod_col_slab(4, 3, dg)
            mod_col_slab(3, 2, dg)
        if "x1" in debug:
            xd = AA.alloc(F32, [128, 2048])
            dma_sp(xd, x1s[0:128, :], X1KEYS, [xd], "xd")
            dump("x1", xd, [128, 2048], F32)
            dump("xh", xh[0:2, :], [2, 2048], F32)
        if stop_after == "F":
            return finish()

        ts(A2c, modc[:, 3, :], 1.0, None, ALU.add)
        tt(A2c, A2c, g2c, ALU.mult)
        B2c = modc[:, 2, :]
        h2T = carve(hT_t, 32768, BF16, [128, NKC, 1024])
        HA.off = 0
        xt2 = [HA.alloc(F32, [128, 2048]) for _ in range(2)]
        junk2 = HA.alloc(BF16, [128, 2048])
        assert HA.off <= 32768
        for t in range(8):
            xt = xt2[t % 2]
            dma_sp(xt, x1s[t * 128:(t + 1) * 128, :], X1KEYS, [xt], ("xt2", t % 2))
            norm_tile(xt, junk2, 128, t, A2c, B2c, lambda dc, t=t: h2T[:, dc, t * 128:(t + 1) * 128])
        norm_tile(xh[0:2, :], junk2, 2, 16, A2c, B2c, lambda dc: h2Th[:, dc, :])
        dump("h2T", h2T, [128, NKC, 1024], BF16)
        dump("h2Th", h2Th, [128, NKC, 2], BF16)
        if stop_after == "G":
            return finish()

        actT = carve(A_t, 0, BF16, [128, NFC, 1024])
        HA.off = 0
        cwt = HA.alloc(F32, [128, 88, 3])
        cbt = HA.alloc(F32, [128, 88])
        uv = [HA.alloc(F32, [128, 1024]) for _ in range(2)]
        ug = [HA.alloc(F32, [128, 1024]) for _ in range(2)]
        sgt = [HA.alloc(F32, [128, 1024]) for _ in range(2)]
        s_rep2 = HA.alloc(BF16, [128, 16, 128])
        assert HA.off <= 32768, HA.off
        g2bc = carve(A_t, 90112, F32, [128, 2048])
        build_s_rep(s_rep2)
        dma_sp(cwt, din["cw"], [], [cwt], "cwt")
        dma_sp(cbt, din["cb"], [], [cbt], "cbt")

        def conv(dst, psl, ph, ch):
            w0, w1, w2 = cwt[:, ch, 0:1], cwt[:, ch, 1:2], cwt[:, ch, 2:3]
            for th in range(2):
                o = dst[:, th * 512:(th + 1) * 512]
                ts(o, psl[th], w2, cbt[:, ch:ch + 1], ALU.mult, ALU.add)
            for th in range(2):
                b0 = th * 512
                stt(dst[:, b0 + 1:b0 + 512], psl[th][:, 0:511], w1, dst[:, b0 + 1:b0 + 512], ALU.mult, ALU.add)
                stt(dst[:, b0 + 2:b0 + 512], psl[th][:, 0:510], w0, dst[:, b0 + 2:b0 + 512], ALU.mult, ALU.add)
            stt(dst[:, 512:513], psl[0][:, 511:512], w1, dst[:, 512:513], ALU.mult, ALU.add)
            stt(dst[:, 512:514], psl[0][:, 510:512], w0, dst[:, 512:514], ALU.mult, ALU.add)
            stt(dst[:, 0:1], ph[:, 1:2], w1, dst[:, 0:1], ALU.mult, ALU.add)
            stt(dst[:, 0:2], ph[:, 0:2], w0, dst[:, 0:2], ALU.mult, ALU.add)

        def up_chunk(sl, m, ch, dst, hi):
            ps2 = [proj_fm(sl, m, lambda kc, th=th: h2T[:, kc, th * 512:(th + 1) * 512]) for th in range(2)]
            psh = proj_fm(sl, m, lambda kc: h2Th[:, kc, :], w=2)
            ts(Phs[:, hi, :], psh[:, 0:2], flag[:, 0:1], None, ALU.mult)
            conv(dst, ps2, Phs[:, hi, :], ch)

        fctr = [0]
        for fp in range(22):
            g2sched = {1: 0, 4: 1, 7: 2, 10: 3, 13: 4, 16: 5, 19: 6, 21: 7}
            if fp in g2sched:
                mod_row_slab(5, g2bc, s_rep2, g2sched[fp])
            slv = slab_load(din["w_up"], 0, NKC, fp * 256)
            slg = slab_load(din["w_up"], 0, NKC, FFN + fp * 256)
            for m in range(2):
                fc = fp * 2 + m
                i = fctr[0] % 2
                fctr[0] += 1
                up_chunk(slv, m, fc, uv[i], 0)
                up_chunk(slg, m, 44 + fc, ug[i], 1)
                act(sgt[i], ug[i], AF.Silu)
                tt(actT[:, fc, :], uv[i], sgt[i], ALU.mult)
        dump("actT", actT, [128, NFC, 1024], BF16)
        if stop_after == "H":
            return finish()

        HA.off = 0
        btmp2 = HA.alloc(F32, [128, 2048])
        xo = [HA.alloc(F32, [128, 8, 256]) for _ in range(2)]
        assert HA.off <= 32768
        dma_sp(btmp2, din["b_g2"], [], [btmp2], "btmp2")
        tt(g2bc, g2bc, btmp2, ALU.add)
        ykeys = []
        for dg in range(8):
            dsl = slice(dg * 256, (dg + 1) * 256)
            xi = xo[dg % 2]
            dma_sp(xi, x1s[:, dsl].rearrange("(t p) n -> p t n", p=128), X1KEYS, [xi], ("xo", dg % 2))
            for (k0, nk) in [(0, 16), (16, 16), (32, 12)]:
                sl = slab_load(din["w_down"], k0 * 128, nk, dg * 256)
                for t in range(8):
                    o = psb[t][:, 0:256]
                    for kk in range(nk):
                        kc = k0 + kk
                        mm(o, actT[:, kc, t * 128:(t + 1) * 128], sl[:, kk, :], kc == 0, kc == NFC - 1)
            for t in range(8):
                ps = psb[t]
                tt(ps[:, 0:256], ps[:, 0:256], g2bc[:, dsl], ALU.mult)
                tt(xi[:, t, :], xi[:, t, :], ps[:, 0:256], ALU.add)
            dma_sp(y[:, dsl].rearrange("(t p) n -> p t n", p=128), xi, [xi], [("y", dg)], ("yw", dg))
            ykeys.append(("yw", dg))
        return finish(extra=ykeys)


_CACHE = {}


def input_names(nc):
    out = []
    for a in nc.m.functions[0].allocations:
        if isinstance(a, mybir.MemoryLocationSet) and a.kind == "ExternalInput":
            out.append(a.memorylocations[0].name)
    return out


def kernel(**inputs):
    inputs = {k: np.asarray(v) for k, v in inputs.items()}
    if "nc" not in _CACHE:
        _CACHE["nc"] = build_program()[0]
        _CACHE["names"] = input_names(_CACHE["nc"])
    nc = _CACHE["nc"]
    names = set(_CACHE["names"])
    in_maps = [{k: v for k, v in make_core_inputs(inputs, c).items() if k in names} for c in range(8)]
    res = run_bass_kernel_spmd(nc, in_maps, core_ids=list(range(8)))
    out = np.empty((4, 2048, D), np.float32)
    for c in range(8):
        out[c // 2, (c % 2) * 1024:(c % 2 + 1) * 1024, :] = res.results[c]["y"]
    return out
```
